# Optimizing a Trainium2 kernel written in Bass

```python
import math
import jax
import jax.numpy as jnp
from jax import lax
import numpy as np

D_MODEL = 1024
BATCH = 32
SEQ = 2048
DEPTH = 1

GRID_W = 64
CTX_LEN = 256

M_HEADS = 8
M_QK_DIM = 64
M_V_DIM = 128
M_QK = M_HEADS * M_QK_DIM
M_V = M_HEADS * M_V_DIM
M_CHUNK = 64

H_HEADS = 8
H_K_DIM = 128
H_V_DIM = 128
H_K = H_HEADS * H_K_DIM
H_V = H_HEADS * H_V_DIM
H_CHUNK = 16

FFN_HIDDEN = 2816
N_MOD = 9
EPS = 1e-6

PROJ_SIZES = (M_QK, M_QK, M_V, M_V, 2 * M_HEADS, 2 * M_HEADS,
              H_K, 2 * H_K, H_V, H_V, D_MODEL, D_MODEL)

kernel_name = "hybrid_mlstm_hgrn2_macaron_dit"


def _rms(x, g):
    xf = x.astype(jnp.float32)
    y = xf * lax.rsqrt(jnp.mean(xf * xf, axis=-1, keepdims=True) + EPS)
    return y.astype(x.dtype) * g


def _head_rms(y, g, heads):
    B, T, W = y.shape
    return _rms(y.reshape(B, T, heads, W // heads), g.reshape(heads, W // heads)).reshape(B, T, W)


def _modulate(xn, shift, scale):
    return xn * (1.0 + scale) + shift


def _swiglu(xm, w_in, w_out):
    a, u = jnp.split(xm @ w_in, 2, axis=-1)
    return (jax.nn.silu(a) * u) @ w_out


def _split_proj(p):
    idx = np.cumsum(PROJ_SIZES)[:-1].tolist()
    return jnp.split(p, idx, axis=-1)


def _to_chunks(a, L):
    B, H, T = a.shape[:3]
    rest = a.shape[3:]
    return jnp.moveaxis(a.reshape(B, H, T // L, L, *rest), 2, 0)


def _from_chunks(a):
    NC, B, H, L = a.shape[:4]
    rest = a.shape[4:]
    return jnp.moveaxis(a, 0, 2).reshape(B, H, NC * L, *rest)


def _to_col_major(a):
    B, T, C = a.shape
    rows = T // GRID_W
    return a.reshape(B, rows, GRID_W, C).transpose(0, 2, 1, 3).reshape(B, T, C)


def _to_row_major(a):
    B, T, C = a.shape
    rows = T // GRID_W
    return a.reshape(B, GRID_W, rows, C).transpose(0, 2, 1, 3).reshape(B, T, C)


def _mlstm_scan(q, k, v, ig, fg, state):
    L = M_CHUNK
    mask = jnp.tril(jnp.ones((L, L), dtype=bool))
    scale = M_QK_DIM ** -0.5
    f32 = jnp.float32
    xs = (_to_chunks(q.astype(f32) * scale, L), _to_chunks(k.astype(f32), L),
          _to_chunks(v.astype(f32), L), _to_chunks(ig.astype(f32), L),
          _to_chunks(jax.nn.log_sigmoid(fg.astype(f32)), L))

    def body(carry, inp):
        C, n, m = carry
        qc, kc, vc, ic, lf = inp
        b = jnp.cumsum(lf, axis=-1)
        dlog = jnp.where(mask, b[..., :, None] - b[..., None, :] + ic[..., None, :], -jnp.inf)
        inter_log = b + m[..., None]
        m_t = jnp.maximum(inter_log, jnp.max(dlog, axis=-1))
        s = jnp.einsum('bhtd,bhsd->bhts', qc, kc) * jnp.exp(dlog - m_t[..., None])
        w_inter = jnp.exp(inter_log - m_t)
        num = w_inter[..., None] * jnp.einsum('bhtd,bhdv->bhtv', qc, C) + jnp.einsum('bhts,bhsv->bhtv', s, vc)
        den = w_inter * jnp.einsum('bhtd,bhd->bht', qc, n) + jnp.sum(s, axis=-1)
        h = num / jnp.maximum(jnp.abs(den), jnp.exp(-m_t))[..., None]
        b_last = b[..., -1]
        w_log = b_last[..., None] - b + ic
        m_new = jnp.maximum(b_last + m, jnp.max(w_log, axis=-1))
        decay = jnp.exp(b_last + m - m_new)
        w = jnp.exp(w_log - m_new[..., None])
        C_new = decay[..., None, None] * C + jnp.einsum('bhs,bhsd,bhsv->bhdv', w, kc, vc)
        n_new = decay[..., None] * n + jnp.einsum('bhs,bhsd->bhd', w, kc)
        return (C_new, n_new, m_new), h

    state, h = lax.scan(body, state, xs)
    return _from_chunks(h), state


def _hgrn_scan(q, k, v, logf, S):
    L = H_CHUNK
    mask = jnp.tril(jnp.ones((L, L), dtype=bool))[:, :, None]
    f32 = jnp.float32
    xs = tuple(_to_chunks(a.astype(f32), L) for a in (q, k, v, logf))

    def body(S, inp):
        qc, kc, vc, lf = inp
        b = jnp.cumsum(lf, axis=2)
        diff = b[:, :, :, None, :] - b[:, :, None, :, :]
        decay = jnp.exp(jnp.where(mask, diff, -jnp.inf))
        attn = jnp.einsum('bhtd,bhtsd,bhsd->bhts', qc, decay, kc)
        o = jnp.einsum('bhtd,bhdv->bhtv', qc * jnp.exp(b), S) + jnp.einsum('bhts,bhsv->bhtv', attn, vc)
        b_last = b[:, :, -1]
        S_new = jnp.exp(b_last)[..., None] * S + jnp.einsum(
            'bhsd,bhsv->bhdv', kc * jnp.exp(b_last[:, :, None, :] - b), vc)
        return S_new, o

    S, o = lax.scan(body, S, xs)
    return _from_chunks(o), S


def _flip_t(args):
    return tuple(jnp.flip(a, axis=2) for a in args)


def _bidir_scan(scan_fn, ctx_fwd, ctx_bwd, lat_fwd, lat_bwd, init):
    hcf, s_f = scan_fn(*ctx_fwd, init)
    hlf, _ = scan_fn(*lat_fwd, s_f)
    hcb, s_b = scan_fn(*_flip_t(ctx_bwd), init)
    hlb, _ = scan_fn(*_flip_t(lat_bwd), s_b)
    return hcf + jnp.flip(hcb, axis=2), hlf + jnp.flip(hlb, axis=2)


def _mlstm_branch(seg_c, seg_l, gain, dtype):
    f32 = jnp.float32

    def prep(seg):
        q, k, v, o, ig, fg = seg
        B, T, _ = q.shape
        hd = lambda a: a.reshape(B, T, M_HEADS, -1).transpose(0, 2, 1, 3)
        ig = ig.astype(f32).transpose(0, 2, 1)
        fg = fg.astype(f32).transpose(0, 2, 1)
        qh, kh, vh = hd(q), hd(k), hd(v)
        fwd = (qh, kh, vh, ig[:, :M_HEADS], fg[:, :M_HEADS])
        bwd = (qh, kh, vh, ig[:, M_HEADS:], fg[:, M_HEADS:])
        return fwd, bwd, o

    cf, cb, oc = prep(seg_c)
    lf, lb, ol = prep(seg_l)
    B = oc.shape[0]
    init = (jnp.zeros((B, M_HEADS, M_QK_DIM, M_V_DIM), f32),
            jnp.zeros((B, M_HEADS, M_QK_DIM), f32),
            jnp.zeros((B, M_HEADS), f32))
    hc, hl = _bidir_scan(_mlstm_scan, cf, cb, lf, lb, init)

    def out(h, o):
        B_, H, T, dv = h.shape
        h = h.transpose(0, 2, 1, 3).reshape(B_, T, H * dv)
        return (_head_rms(h, gain, M_HEADS) * jax.nn.sigmoid(o.astype(f32))).astype(dtype)

    return out(hc, oc), out(hl, ol)


def _hgrn_gates(f_pre, lb):
    fp = f_pre.astype(jnp.float32)
    logf = jnp.log(lb + (1.0 - lb) * jax.nn.sigmoid(fp))
    k = (1.0 - lb) * jax.nn.sigmoid(-fp)
    return logf, k


def _hgrn_branch(seg_c, seg_l, lb, gain, dtype):
    f32 = jnp.float32

    def prep(seg):
        q, f, i, g = seg
        B, T, _ = q.shape
        hd = lambda a: a.astype(f32).reshape(B, T, H_HEADS, -1).transpose(0, 2, 1, 3)
        qh = hd(jax.nn.silu(q.astype(f32)))
        vh = hd(i)
        lf_f, k_f = _hgrn_gates(f[..., :H_K], lb[0])
        lf_b, k_b = _hgrn_gates(f[..., H_K:], lb[1])
        return (qh, hd(k_f), vh, hd(lf_f)), (qh, hd(k_b), vh, hd(lf_b)), g

    cf, cb, gc = prep(seg_c)
    lf, lbw, gl = prep(tuple(_to_col_major(a) for a in seg_l))
    B = gc.shape[0]
    init = jnp.zeros((B, H_HEADS, H_K_DIM, H_V_DIM), f32)
    hc, hl = _bidir_scan(_hgrn_scan, cf, cb, lf, lbw, init)

    def out(h, g):
        B_, H, T, dv = h.shape
        h = h.transpose(0, 2, 1, 3).reshape(B_, T, H * dv)
        return (_head_rms(h, gain, H_HEADS) * jax.nn.silu(g.astype(f32))).astype(dtype)

    return out(hc, gc), _to_row_major(out(hl, gl))


def _token_mix(xc, xl, w_in, b_in, m_gain, lb, h_gain, proj_m, proj_h, w_out, need_ctx):
    seg_c = _split_proj(xc @ w_in + b_in)
    seg_l = _split_proj(xl @ w_in + b_in)
    hm_c, hm_l = _mlstm_branch(seg_c[0:6], seg_l[0:6], m_gain, xl.dtype)
    hh_c, hh_l = _hgrn_branch(seg_c[6:10], seg_l[6:10], lb, h_gain, xl.dtype)

    def merge(hm, hh, gm, gh):
        return (jax.nn.sigmoid(gm) * (hm @ proj_m) + jax.nn.sigmoid(gh) * (hh @ proj_h)) @ w_out

    y_l = merge(hm_l, hh_l, seg_l[10], seg_l[11])
    y_c = merge(hm_c, hh_c, seg_c[10], seg_c[11]) if need_ctx else None
    return y_c, y_l


def setup_inputs(seed: int = 0) -> dict:
    key = jax.random.key(seed)
    ks = jax.random.split(key, 24)
    D, F = D_MODEL, FFN_HIDDEN
    P = int(sum(PROJ_SIZES))
    nrm = lambda k, shape, s: jax.random.normal(k, shape, jnp.float32) * s
    gains = lambda k, shape: 1.0 + nrm(k, shape, 0.05)
    f_start = int(sum(PROJ_SIZES[:5]))
    f_bias = jnp.tile(jnp.linspace(3.0, 6.0, M_HEADS), 2)
    b_off = jnp.zeros((P,), jnp.float32).at[f_start:f_start + 2 * M_HEADS].set(f_bias)
    return {
        "x": nrm(ks[0], (BATCH, SEQ, D), 1.0),
        "c": nrm(ks[1], (BATCH, D), 1.0),
        "ctx": nrm(ks[2], (BATCH, CTX_LEN, D), 1.0),
        "c_ctx": nrm(ks[3], (D,), 1.0),
        "ada_w": nrm(ks[4], (DEPTH, D, N_MOD * D), D ** -0.5),
        "ada_b": nrm(ks[5], (DEPTH, N_MOD * D), 0.02),
        "ffn1_norm": gains(ks[6], (DEPTH, D)),
        "ffn1_w_in": nrm(ks[7], (DEPTH, D, 2 * F), D ** -0.5),
        "ffn1_w_out": nrm(ks[8], (DEPTH, F, D), F ** -0.5),
        "mix_norm": gains(ks[9], (DEPTH, D)),
        "mix_w_in": nrm(ks[10], (DEPTH, D, P), D ** -0.5),
        "mix_b_in": b_off[None, :] + nrm(ks[11], (DEPTH, P), 0.02),
        "mlstm_norm": gains(ks[12], (DEPTH, M_V)),
        "hgrn_lb_logits": nrm(ks[13], (2, DEPTH + 1, H_K), 0.5),
        "hgrn_norm": gains(ks[14], (DEPTH, H_V)),
        "proj_m": nrm(ks[15], (DEPTH, M_V, D), M_V ** -0.5),
        "proj_h": nrm(ks[16], (DEPTH, H_V, D), H_V ** -0.5),
        "mix_w_out": nrm(ks[17], (DEPTH, D, D), D ** -0.5),
        "ffn2_norm": gains(ks[18], (DEPTH, D)),
        "ffn2_w_in": nrm(ks[19], (DEPTH, D, 2 * F), D ** -0.5),
        "ffn2_w_out": nrm(ks[20], (DEPTH, F, D), F ** -0.5),
        "final_norm": gains(ks[21], (D,)),
    }


def reference(x, c, ctx, c_ctx, ada_w, ada_b, ffn1_norm, ffn1_w_in, ffn1_w_out, mix_norm,
              mix_w_in, mix_b_in, mlstm_norm, hgrn_lb_logits, hgrn_norm, proj_m, proj_h,
              mix_w_out, ffn2_norm, ffn2_w_in, ffn2_w_out, final_norm):
    B = x.shape[0]
    D = x.shape[-1]
    lb_all = jnp.cumsum(jax.nn.softmax(hgrn_lb_logits.astype(jnp.float32), axis=1), axis=1)
    h = ctx
    for l in range(DEPTH):
        last = l == DEPTH - 1
        ml = (jax.nn.silu(c) @ ada_w[l] + ada_b[l]).reshape(B, N_MOD, 1, D)
        mc = (jax.nn.silu(c_ctx) @ ada_w[l] + ada_b[l]).reshape(N_MOD, 1, 1, D)
        x = x + 0.5 * ml[:, 2] * _swiglu(_modulate(_rms(x, ffn1_norm[l]), ml[:, 0], ml[:, 1]), ffn1_w_in[l], ffn1_w_out[l])
        h = h + 0.5 * mc[2] * _swiglu(_modulate(_rms(h, ffn1_norm[l]), mc[0], mc[1]), ffn1_w_in[l], ffn1_w_out[l])
        y_c, y_l = _token_mix(_modulate(_rms(h, mix_norm[l]), mc[3], mc[4]),
                              _modulate(_rms(x, mix_norm[l]), ml[:, 3], ml[:, 4]),
                              mix_w_in[l], mix_b_in[l], mlstm_norm[l], lb_all[:, l], hgrn_norm[l],
                              proj_m[l], proj_h[l], mix_w_out[l], not last)
        x = x + ml[:, 5] * y_l
        if not last:
            h = h + mc[5] * y_c
            h = h + 0.5 * mc[8] * _swiglu(_modulate(_rms(h, ffn2_norm[l]), mc[6], mc[7]), ffn2_w_in[l], ffn2_w_out[l])
        x = x + 0.5 * ml[:, 8] * _swiglu(_modulate(_rms(x, ffn2_norm[l]), ml[:, 6], ml[:, 7]), ffn2_w_in[l], ffn2_w_out[l])
    return _rms(x, final_norm)
```

```python
import numpy as np
import concourse.bass as bass
import concourse.mybir as mybir
from concourse.bass_utils import run_bass_kernel_spmd
from contextlib import ExitStack

F32 = mybir.dt.float32
BF16 = mybir.dt.bfloat16
AF = mybir.ActivationFunctionType
ALU = mybir.AluOpType
AX = mybir.AxisListType

D = 1024
FH = 2816
NI = 22
EPS = 1e-6
NCORES = 8
STAGE = 3
HG_ACT_EVAC = False
HG_PIPE = True
HG_SEQ = 1
HG_POOL_CAST = False
FORCE_LAST = False


class Buf:
    __slots__ = ("t", "w", "r", "name")

    def __init__(self, t, name=""):
        self.t = t
        self.w = []
        self.r = []
        self.name = name

    def __getitem__(self, idx):
        return self.t[idx]


class Prog:
    def __init__(self, nc, n_dma_ch=8):
        self.nc = nc
        self.es = ExitStack()
        self.eng = {"pe": nc.tensor, "act": nc.scalar, "dve": nc.vector, "pool": nc.gpsimd, "sp": nc.sync}
        self.sem = {}
        self.cnt = {}
        for e in ("pe", "act", "dve", "pool"):
            self.sem[e] = self.es.enter_context(nc.semaphore("s_" + e))
            self.cnt[e] = 0
        self.ch = {}
        self.chi = {}
        for q in ("sp", "pool"):
            lst = []
            for i in range(n_dma_ch):
                k = "d_%s%d" % (q, i)
                self.sem[k] = self.es.enter_context(nc.semaphore(k))
                self.cnt[k] = 0
                lst.append(k)
            self.ch[q] = lst
            self.chi[q] = 0
        self.seen = {e: {} for e in self.eng}
        self.n_inst = 0
        self.n_wait = 0
        self.rr = 0

    def sb(self, name, shape, dt, stack=None):
        self.uid = getattr(self, "uid", 0) + 1
        name = "%s_u%d" % (name, self.uid)
        t = (stack or self.es).enter_context(self.nc.sbuf_tensor(name, list(shape), dt))
        return Buf(t, name)

    def ps(self, name, shape, dt, stack=None):
        t = (stack or self.es).enter_context(self.nc.psum_tensor(name, list(shape), dt))
        return Buf(t, name)

    def dram(self, name, shape, dt, kind="Internal"):
        t = self.nc.dram_tensor(name, list(shape), dt, kind=kind)
        return Buf(t.ap(), name)

    def _wait(self, e, deps):
        need = {}
        for (k, v) in deps:
            if v > need.get(k, 0):
                need[k] = v
        seen = self.seen[e]
        for k, v in need.items():
            if k == "pe" and e == "pe":
                continue
            if seen.get(k, 0) >= v:
                continue
            self.eng[e].wait_ge(self.sem[k], v)
            self.n_wait += 1
            seen[k] = v

    def _deps(self, reads, writes):
        deps = []
        for b in reads:
            deps.extend(b.w)
        for b in writes:
            deps.extend(b.w)
            deps.extend(b.r)
        return deps

    def _record(self, tick, reads, writes):
        writes = [b for b in writes if b.name != "junk"]
        for b in reads:
            b.r.append(tick)
            if len(b.r) > 48:
                m = {}
                for (k, v) in b.r:
                    if v > m.get(k, 0):
                        m[k] = v
                b.r = list(m.items())
        for b in writes:
            b.w = [tick]
            b.r = []

    def op(self, e, fn, reads=(), writes=(), **kw):
        self._wait(e, self._deps(reads, writes))
        ins = fn(**kw)
        self.cnt[e] += 1
        ins.then_inc(self.sem[e], 1)
        self._record((e, self.cnt[e]), reads, writes)
        self.n_inst += 1
        return ins

    def mm(self, out_buf, reads, last=True, transpose=False, **kw):
        e = "pe"
        if FORCE_LAST:
            last = True
        self._wait(e, self._deps(reads, [out_buf]))
        if transpose:
            ins = self.nc.tensor.transpose(**kw)
        else:
            ins = self.nc.tensor.matmul(**kw)
        self._record((e, self.cnt[e] + 1), reads, [out_buf])
        self.n_inst += 1
        if last:
            self.cnt[e] += 1
            ins.then_inc(self.sem[e], 1)
        return ins

    def dma(self, q, out_buf, out_ap, in_buf, in_ap, **kw):
        chs = self.ch[q]
        k = chs[self.chi[q] % len(chs)]
        self.chi[q] += 1
        deps = self._deps([in_buf], [out_buf])
        if self.cnt[k] > 0:
            deps.append((k, self.cnt[k]))
        self._wait(q, deps)
        ins = self.eng[q].dma_start(out=out_ap, in_=in_ap, **kw)
        self.cnt[k] += 16
        ins.then_inc(self.sem[k], 16)
        self._record((k, self.cnt[k]), [in_buf], [out_buf])
        self.n_inst += 1
        return ins

    def inherit(self, new, olds):
        for o in olds:
            new.w = list(new.w) + list(o.w)
            new.r = list(new.r) + list(o.r)

    def barrier(self):
        allk = [(k, v) for k, v in self.cnt.items() if v > 0]
        for e in self.eng:
            self._wait(e, allk)

    def ve(self):
        self.rr += 1
        return "dve" if (self.rr % 3) else "pool"

    def eng_of(self, e):
        return self.nc.vector if e == "dve" else self.nc.gpsimd

    def tt(self, e, out_b, out, in0_b, in0, in1_b, in1, op):
        self.op(e, self.eng_of(e).tensor_tensor, [in0_b, in1_b], [out_b], out=out, in0=in0, in1=in1, op=op)

    def act(self, out_b, out, in_b, in_, func, extra_reads=(), **kw):
        self.op("act", self.nc.scalar.activation, [in_b] + list(extra_reads), [out_b], out=out, in_=in_, func=func, **kw)


O_MQ, O_MK, O_MV, O_MO, O_IG, O_FG, O_HQ, O_HF, O_HI, O_HG, O_GM, O_GH = (
    0, 512, 1024, 2048, 3072, 3088, 3104, 4128, 6176, 7200, 8224, 9248)

V_BG = 0
V_BMFM = 1
V_BHFM = 9
V_BMG = 41
V_HN = 57
V_LB = 65
NVEC = 97


def build(NB, T, CT, dbg=False):
    GW = 64
    ROWS = T // GW
    TA = CT + T
    LM = 128
    LH = 32
    NBLK = TA // 128
    NBLK_C = CT // 128
    NCHH = TA // LH
    NTL = 512
    NB1 = NB + 1

    nc = bass.Bass("TRN2", target_bir_lowering=False)
    p = Prog(nc)

    def din(name, shape, dt=F32):
        return Buf(nc.dram_tensor(name, list(shape), dt, kind="ExternalInput").ap(), name)

    x_d = din("x", [NB * T, D])
    ctx_d = din("ctx", [NB * CT, D])
    cc_d = din("cc", [128, 8 * NB1])
    adaw_d = din("ada_w", [D, 9 * D])
    adab_d = din("ada_b", [1, 9 * D])
    rows_d = din("rows", [5, D])
    vecs_d = din("vecs", [128, NVEC])
    consts_d = din("consts", [128, 6 * 128])
    cmask_d = din("cmask", [64, TA])
    hmask_d = din("hmask", [128, TA])
    sel_d = din("sel", [8, 8 * 64])
    w1i_d = din("w1i", [NI, 128, 2048])
    w1o_d = din("w1o", [2, 128, NI * 512])
    w2i_d = din("w2i", [NI, 128, 2048])
    w2o_d = din("w2o", [2, 128, NI * 512])
    wg_d = din("wg", [128, 8 * 32])
    wmfm_d = din("wmfm", [4, 128, 8 * 256])
    wmtm_d = din("wmtm", [8, 128, 8 * 320])
    bmtm_d = din("bmtm", [8, 1, 320])
    whfm_d = din("whfm", [8, 128, 8 * 512])
    whtm_d = din("whtm", [2, 128, 8 * 512])
    bhtm_d = din("bhtm", [2, 1, 512])
    wmg_d = din("wmg", [8, 128, 8 * 256])
    wpj_d = din("wpj", [8, 128, 8 * 256])
    wmo_d = din("wmo", [2, 128, 8 * 512])
    out_d = Buf(nc.dram_tensor("out", [NB * T, D], F32, kind="ExternalOutput").ap(), "out")

    modd = p.dram("modd", [NB1, 9 * D], F32)
    x1_s = p.dram("x1_s", [NB * T, D], F32)
    xl_s = p.dram("xl_s", [NB, 128, 8 * TA], BF16)
    gates_s = p.dram("gates_s", [NB, 32, TA], F32)
    hm_s = p.dram("hm_s", [NB, 128, 8 * T], BF16)
    hh_s = p.dram("hh_s", [NB, 128, 8 * T], BF16)
    dbg_out = {}

    cst = p.sb("cst", [128, 6, 128], F32)
    cstb = p.sb("cstb", [128, 6, 128], BF16)
    vecs = p.sb("vecs_sb", [128, NVEC], F32)
    ones1 = p.sb("ones1", [1, 128], BF16)
    p.dma("sp", cst, cst[:], consts_d, consts_d[:, :].rearrange("p (a b) -> p a b", b=128))
    p.dma("sp", vecs, vecs[:], vecs_d, vecs_d[:, :])
    p.op("dve", nc.vector.tensor_copy, [cst], [cstb], out=cstb[:], in_=cst[:])
    p.op("dve", nc.vector.memset, [], [ones1], ap=ones1[:], constant=1.0)
    ident_b = cstb[:, 0, :]
    ones_b = cstb[:, 1, :]

    banks = [p.ps("bank%d" % i, [128, 512], F32) for i in range(6)]
    tbanks = [p.ps("tbank%d" % i, [128, 1024], BF16) for i in range(2)]
    bstate = {"i": 0, "t": 0}

    def bank():
        bstate["i"] += 1
        return banks[bstate["i"] % len(banks)]

    def tbank():
        bstate["t"] += 1
        return tbanks[bstate["t"] % 2]

    w1i_b = p.dram("w1i_b", [NI, 128, 2048], BF16)
    w1o_b = p.dram("w1o_b", [2, 128, NI * 512], BF16)
    w2i_b = p.dram("w2i_b", [NI, 128, 2048], BF16)
    w2o_b = p.dram("w2o_b", [2, 128, NI * 512], BF16)
    wmg_b = p.dram("wmg_b", [8, 128, 2048], BF16)
    wpj_b = p.dram("wpj_b", [8, 128, 2048], BF16)
    wmo_b = p.dram("wmo_b", [2, 128, 4096], BF16)

    PC = {"gen": None}

    def precast_gen(stk, which):
        stg = [p.sb("stg%d" % i, [128, 2816], BF16, stk) for i in range(4)]
        si_ = [0]

        def one(src, dst, idx, n):
            c0 = 0
            while c0 < n:
                w_ = min(2816, n - c0)
                st = stg[si_[0] % 4]
                si_[0] += 1
                p.dma("pool", st, st[:, 0:w_], src, src[idx, :, c0:c0 + w_])
                p.dma("sp", dst, dst[idx, :, c0:c0 + w_], st, st[:, 0:w_])
                c0 += w_
                yield
        if which == 0:
            for i in range(NI):
                yield from one(w1i_d, w1i_b, i, 2048)
            for nh in range(2):
                yield from one(w1o_d, w1o_b, nh, NI * 512)
        else:
            for mc in range(8):
                yield from one(wmg_d, wmg_b, mc, 2048)
                yield from one(wpj_d, wpj_b, mc, 2048)
            for nh in range(2):
                yield from one(wmo_d, wmo_b, nh, 4096)
            for i in range(NI):
                yield from one(w2i_d, w2i_b, i, 2048)
            for nh in range(2):
                yield from one(w2o_d, w2o_b, nh, NI * 512)

    def pump(n=1):
        g = PC["gen"]
        if g is None:
            return
        for _ in range(n):
            try:
                next(g)
            except StopIteration:
                PC["gen"] = None
                return

    with ExitStack() as ph:
        PC["gen"] = precast_gen(ph, 0)
        pump(1000)
        ccs = p.sb("ccs", [128, 8, NB1], F32, ph)
        scb = p.sb("scb", [128, 8, NB1], BF16, ph)
        p.dma("sp", ccs, ccs[:], cc_d, cc_d[:, :].rearrange("p (k n) -> p k n", n=NB1))
        p.act(scb, scb[:], ccs, ccs[:], AF.Silu)
        awr = [p.sb("awr%d" % i, [128, 8, 512], BF16, ph) for i in range(2)]
        abr = [p.sb("abr%d" % i, [1, 512], BF16, ph) for i in range(2)]
        mrow = [p.sb("mrow%d" % i, [NB1, 512], F32, ph) for i in range(2)]
        adaw_v = adaw_d[:, :].rearrange("(k p) n -> p k n", p=128)
        for n in range(18):
            aw = awr[n % 2]
            ab = abr[n % 2]
            mr = mrow[n % 2]
            p.dma("pool", aw, aw[:], adaw_d, adaw_v[:, :, n * 512:(n + 1) * 512])
            p.dma("pool", ab, ab[:], adab_d, adab_d[:, n * 512:(n + 1) * 512])
            bk = bank()
            for k in range(8):
                p.mm(bk, [scb, aw], last=False, out=bk[0:NB1, :], lhsT=scb[:, k, :], rhs=aw[:, k, :], start=(k == 0), stop=False)
            p.mm(bk, [ones1, ab], out=bk[0:NB1, :], lhsT=ones1[0:1, 0:NB1], rhs=ab[0:1, :], start=False, stop=True)
            p.op("dve", nc.vector.tensor_copy, [bk], [mr], out=mr[:], in_=bk[0:NB1, :])
            p.dma("sp", modd, modd[:, n * 512:(n + 1) * 512], mr, mr[:])
        p.barrier()

    FB = {}
    cnt = {"wi": 0, "wo": 0, "tmp": 0, "sil": 0, "t2": 0}

    def alloc_ffn(ffs, tag):
        FB["xts"] = [p.sb("xt%d%s" % (i, tag), [128, 4, D], F32, ffs) for i in range(2)]
        FB["junk"] = p.sb("junk" + tag, [128, D], BF16, ffs)
        FB["junk"].name = "junk"
        FB["tmps"] = [p.sb("tmp%d%s" % (i, tag), [128, D], F32, ffs) for i in range(2)]
        FB["xm"] = p.sb("xm" + tag, [128, 4, D], BF16, ffs)
        FB["xmT"] = p.sb("xmT" + tag, [128, 8, NTL], BF16, ffs)
        FB["gbf"] = p.sb("gbf" + tag, [128, NI, NTL], BF16, ffs)
        FB["sils"] = [p.sb("sil%d%s" % (i, tag), [128, NTL], F32, ffs) for i in range(2)]
        FB["t2s"] = [p.sb("t2_%d%s" % (i, tag), [128, 512], F32, ffs) for i in range(2)]
        FB["wir"] = [p.sb("wir%d%s" % (i, tag), [128, 8, 256], BF16, ffs) for i in range(4)]
        FB["wor"] = [p.sb("wor%d%s" % (i, tag), [128, 11, 512], BF16, ffs) for i in range(3)]
        FB["ss"] = p.sb("ss" + tag, [128, 8], F32, ffs)
        FB["rstd"] = p.sb("rstd" + tag, [128, 8], F32, ffs)
        FB["bct"] = {k: p.sb("bc_" + k + tag, [128, D], F32, ffs) for k in ("gs", "sh", "gate", "gs2", "sh2", "nrm")}

    def bc_load(dst, src_buf, src_ap):
        p.dma("sp", dst, dst[:], src_buf, src_ap.partition_broadcast(128))

    def load_mod(b, i_shift, i_scale, i_gate, nrm_row, gs, sh, gate):
        bc_load(FB["bct"]["nrm"], rows_d, rows_d[nrm_row:nrm_row + 1, :])
        bc_load(gs, modd, modd[b:b + 1, i_scale * D:(i_scale + 1) * D])
        bc_load(sh, modd, modd[b:b + 1, i_shift * D:(i_shift + 1) * D])
        if gate is not None:
            bc_load(gate, modd, modd[b:b + 1, i_gate * D:(i_gate + 1) * D])
        p.op("dve", nc.vector.scalar_tensor_tensor, [gs, FB["bct"]["nrm"]], [gs], out=gs[:], in0=gs[:], scalar=1.0,
             in1=FB["bct"]["nrm"][:], op0=ALU.add, op1=ALU.mult)

    def norm_mod_T(xt, J, gs, sh):
        ss, rstd, junk, tmps, xm, xmT = FB["ss"], FB["rstd"], FB["junk"], FB["tmps"], FB["xm"], FB["xmT"]
        p.op("dve", nc.vector.memset, [], [ss], ap=ss[:], constant=0.0)
        for j in range(J):
            p.act(junk, junk[:], xt, xt[:, j, :], AF.Square, extra_reads=[ss], accum_out=ss[:, j:j + 1])
            ss.w = [("act", p.cnt["act"])]
        p.act(rstd, rstd[:, 0:J], ss, ss[:, 0:J], AF.Sqrt, scale=1.0 / D, bias=EPS)
        p.op("dve", nc.vector.reciprocal, [rstd], [rstd], out=rstd[:, 0:J], in_=rstd[:, 0:J])
        for j in range(J):
            cnt["tmp"] += 1
            tm = tmps[cnt["tmp"] % 2]
            p.op("dve", nc.vector.scalar_tensor_tensor, [xt, rstd, gs], [tm], out=tm[:], in0=xt[:, j, :],
                 scalar=rstd[:, j:j + 1], in1=gs[:], op0=ALU.mult, op1=ALU.mult)
            p.tt("pool", xm, xm[:, j, :], tm, tm[:], sh, sh[:], ALU.add)
        for kc in range(8):
            tb = tbank()
            for j in range(J):
                p.mm(tb, [xm, cstb], last=(j == J - 1), transpose=True, out=tb[:, j * 128:(j + 1) * 128],
                     in_=xm[:, j, kc * 128:(kc + 1) * 128], identity=ident_b)
            if kc % 2 == 0:
                p.act(xmT, xmT[:, kc, 0:J * 128], tb, tb[:, 0:J * 128], AF.Copy)
            else:
                p.op("dve", nc.vector.tensor_copy, [tb], [xmT], out=xmT[:, kc, 0:J * 128], in_=tb[:, 0:J * 128])

    def ffn(xt, J, gate, wi_d, wo_d):
        NT = J * 128
        wir, wor, xmT, gbf, sils, t2s = FB["wir"], FB["wor"], FB["xmT"], FB["gbf"], FB["sils"], FB["t2s"]
        for i in range(NI):
            cnt["wi"] += 1
            wt = wir[cnt["wi"] % 4]
            p.dma("sp", wt, wt[:], wi_d, wi_d[i, :, :].rearrange("p (k c) -> p k c", c=256))
            ba = bank()
            for k in range(8):
                p.mm(ba, [wt, xmT], last=(k == 7), out=ba[:, 0:NT], lhsT=wt[:, k, 0:128], rhs=xmT[:, k, 0:NT], start=(k == 0), stop=(k == 7))
            bu = bank()
            for k in range(8):
                p.mm(bu, [wt, xmT], last=(k == 7), out=bu[:, 0:NT], lhsT=wt[:, k, 128:256], rhs=xmT[:, k, 0:NT], start=(k == 0), stop=(k == 7))
            cnt["sil"] += 1
            sl = sils[cnt["sil"] % 2]
            p.act(sl, sl[:, 0:NT], ba, ba[:, 0:NT], AF.Silu)
            p.op("dve", nc.vector.tensor_tensor, [sl, bu], [gbf], out=gbf[:, i, 0:NT], in0=sl[:, 0:NT], in1=bu[:, 0:NT], op=ALU.mult)
            if i % 2 == 0:
                pump(1)
        for nh in range(2):
            bos = [bank() for _ in range(J)]
            for ih in range(2):
                cnt["wo"] += 1
                wo = wor[cnt["wo"] % len(wor)]
                p.dma("sp", wo, wo[:], wo_d, wo_d[nh, :, ih * 11 * 512:(ih + 1) * 11 * 512].rearrange("p (i c) -> p i c", c=512))
                for j in range(J):
                    bo = bos[j]
                    for i2 in range(11):
                        i = ih * 11 + i2
                        p.mm(bo, [gbf, wo], last=(i2 == 10), out=bo[:, :], lhsT=gbf[:, i, j * 128:(j + 1) * 128], rhs=wo[:, i2, :],
                             start=(i == 0), stop=(i == NI - 1))
            for j in range(J):
                bo = bos[j]
                cnt["t2"] += 1
                t2 = t2s[cnt["t2"] % 2]
                p.op("dve", nc.vector.scalar_tensor_tensor, [bo, gate], [t2], out=t2[:], in0=bo[:, :], scalar=0.5,
                     in1=gate[:, nh * 512:(nh + 1) * 512], op0=ALU.mult, op1=ALU.mult)
                p.tt("pool", xt, xt[:, j, nh * 512:(nh + 1) * 512], xt, xt[:, j, nh * 512:(nh + 1) * 512], t2, t2[:], ALU.add)

    with ExitStack() as ph:
        alloc_ffn(ph, "_a")
        PC["gen"] = precast_gen(ph, 1)
        bct, xts, xmT = FB["bct"], FB["xts"], FB["xmT"]
        wgb = p.sb("wgb", [128, 8, 32], BF16, ph)
        p.dma("pool", wgb, wgb[:], wg_d, wg_d[:, :].rearrange("p (k c) -> p k c", c=32))
        gsb = [p.sb("gsb%d" % i, [32, NTL], F32, ph) for i in range(2)]
        tix = 0
        for b in range(NB):
            for seg in ("ctx", "lat"):
                bm = NB if seg == "ctx" else b
                load_mod(bm, 0, 1, 2, 0, bct["gs"], bct["sh"], bct["gate"])
                load_mod(bm, 3, 4, None, 1, bct["gs2"], bct["sh2"], None)
                ntok = CT if seg == "ctx" else T
                src = ctx_d if seg == "ctx" else x_d
                base = b * ntok
                tok0 = 0
                while tok0 < ntok:
                    NT = min(NTL, ntok - tok0)
                    J = NT // 128
                    tix += 1
                    xt = xts[tix % 2]
                    p.dma("sp", xt, xt[:, 0:J, :], src, src[base + tok0:base + tok0 + NT, :].rearrange("(j p) d -> p j d", p=128))
                    norm_mod_T(xt, J, bct["gs"], bct["sh"])
                    ffn(xt, J, bct["gate"], w1i_b, w1o_b)
                    if seg == "lat":
                        p.dma("sp", x1_s, x1_s[base + tok0:base + tok0 + NT, :].rearrange("(j p) d -> p j d", p=128), xt, xt[:, 0:J, :])
                    norm_mod_T(xt, J, bct["gs2"], bct["sh2"])
                    col0 = tok0 if seg == "ctx" else CT + tok0
                    p.dma("sp", xl_s, xl_s[b, :, :].rearrange("p (k t) -> p k t", t=TA)[:, :, col0:col0 + NT], xmT, xmT[:, :, 0:NT])
                    bk = bank()
                    for k in range(8):
                        p.mm(bk, [wgb, xmT], last=(k == 7), out=bk[0:32, 0:NT], lhsT=wgb[:, k, :], rhs=xmT[:, k, 0:NT], start=(k == 0), stop=(k == 7))
                    gs_ = gsb[tix % 2]
                    p.act(gs_, gs_[:, 0:NT], bk, bk[0:32, 0:NT], AF.Identity, extra_reads=[vecs], bias=vecs[0:32, V_BG:V_BG + 1], scale=1.0)
                    p.dma("sp", gates_s, gates_s[b, :, col0:col0 + NT], gs_, gs_[:, 0:NT])
                    tok0 += NT
        pump(1000)
        p.barrier()

    NCH = NBLK
    NBLK_L = NBLK - NBLK_C
    ttiles = []
    t0_ = 0
    while t0_ < CT:
        n_ = min(NTL, CT - t0_)
        ttiles.append((t0_, n_))
        t0_ += n_
    while t0_ < TA:
        ttiles.append((t0_, NTL))
        t0_ += NTL
    ord_f = list(range(NBLK))
    ord_b = list(range(NBLK_C - 1, -1, -1)) + list(range(NBLK - 1, NBLK_C - 1, -1))
    with ExitStack() as ph:
        GT = p.sb("GT", [128, NBLK, 128], F32, ph)
        decb = p.sb("decb", [128, 64, NCH], F32, ph)
        mgb = p.sb("mgb", [128, D], F32, ph)
        LB = p.sb("LB", [128, 16], F32, ph)
        OML = p.sb("OML", [128, 16], F32, ph)
        NOML = p.sb("NOML", [128, 16], F32, ph)
        xh = p.sb("xh", [128, 8, TA], BF16, ph)
        p.dma("sp", mgb, mgb[:], rows_d, rows_d[4:5, :].partition_broadcast(128))
        lbv = vecs[:, V_LB:V_LB + 32].rearrange("p (d l h) -> p d l h", d=2, l=2)
        p.op("dve", nc.vector.tensor_tensor, [vecs], [LB], out=LB[:].rearrange("p (d h) -> p d h", d=2), in0=lbv[:, :, 0, :], in1=lbv[:, :, 1, :], op=ALU.subtract)
        p.act(LB, LB[:], LB, LB[:], AF.Sigmoid)
        p.op("dve", nc.vector.tensor_scalar, [LB], [OML], out=OML[:], in0=LB[:], scalar1=-1.0, scalar2=1.0, op0=ALU.mult, op1=ALU.add)
        p.op("dve", nc.vector.tensor_scalar, [OML], [NOML], out=NOML[:], in0=OML[:], scalar1=-1.0, scalar2=None, op0=ALU.mult)
        with ExitStack() as g2:
            IG = p.sb("IG", [128, TA], F32, g2)
            FG = p.sb("FG", [128, TA], F32, g2)
            Pc = p.sb("Pc", [128, TA], F32, g2)
            NBq = p.sb("NBq", [128, TA], F32, g2)
            Aa = p.sb("Aa", [128, TA], F32, g2)
            WTH = p.sb("WTH", [128, TA], BF16, g2)
            cmk = p.sb("cmk", [128, TA], F32, g2)
            tot = p.sb("tot", [128, NCH], F32, g2)
            amax = p.sb("amax", [128, NCH], F32, g2)
            mc = p.sb("mc", [128, NCH], F32, g2)
            MP = p.sb("MP", [128, NCH], F32, g2)
            dec = p.sb("dec", [64, NCH], F32, g2)
            dbig = p.sb("dbig", [64, 64, NCH], F32, g2)
            ones64 = p.sb("ones64", [64, 128], F32, g2)
            p.op("dve", nc.vector.memset, [], [IG], ap=IG[:], constant=0.0)
            p.op("dve", nc.vector.memset, [], [FG], ap=FG[:], constant=0.0)
            p.op("dve", nc.vector.memset, [], [ones64], ap=ones64[:], constant=1.0)
            p.dma("sp", cmk, cmk[0:64, :], cmask_d, cmask_d[:, :])
            p.dma("sp", cmk, cmk[64:128, :], cmask_d, cmask_d[:, :])
            for q_ in range(2):
                for dr in range(2):
                    for b in range(NB):
                        r = q_ * 64 + dr * 32 + b * 8
                        p.dma("sp", IG, IG[r:r + 8, :], gates_s, gates_s[b, dr * 8:dr * 8 + 8, :])
                        p.dma("sp", FG, FG[r:r + 8, :], gates_s, gates_s[b, 16 + dr * 8:16 + dr * 8 + 8, :])
            p.act(FG, FG[:], FG, FG[:], AF.Exp, scale=-1.0)
            p.act(FG, FG[:], FG, FG[:], AF.Ln, bias=1.0, scale=1.0)
            p.op("dve", nc.vector.tensor_tensor_scan, [cmk, FG], [Pc], out=Pc[:], data0=cmk[:], data1=FG[:], initial=0.0, op0=ALU.mult, op1=ALU.add)
            Pv = Pc[:].rearrange("p (c l) -> p c l", l=LM)
            p.op("dve", nc.vector.tensor_copy, [Pc], [tot], out=tot[:].rearrange("p (c o) -> p c o", o=1), in_=Pv[:, :, LM - 1:LM])
            for q_ in range(2):
                f0, f1, b0_, b1_ = q_ * 64, q_ * 64 + 32, q_ * 64 + 32, q_ * 64 + 64
                p.op("dve", nc.vector.tensor_copy, [Pc], [NBq], out=NBq[f0:f1, :], in_=Pc[f0:f1, :])
                p.op("dve", nc.vector.tensor_tensor, [FG, Pc], [NBq], out=NBq[b0_:b1_, :], in0=FG[b0_:b1_, :], in1=Pc[b0_:b1_, :], op=ALU.subtract)
                p.op("dve", nc.vector.tensor_tensor, [NBq, tot], [NBq], out=NBq[b0_:b1_, :].rearrange("p (c l) -> p c l", l=LM),
                     in0=NBq[b0_:b1_, :].rearrange("p (c l) -> p c l", l=LM),
                     in1=tot[b0_:b1_, :].rearrange("p (c o) -> p c o", o=1).broadcast_to([32, NCH, LM]), op=ALU.add)
            p.op("dve", nc.vector.tensor_tensor, [IG, NBq], [Aa], out=Aa[:], in0=IG[:], in1=NBq[:], op=ALU.add)
            p.op("dve", nc.vector.tensor_reduce, [Aa], [amax], out=amax[:], in_=Aa[:].rearrange("p (c l) -> p c l", l=LM), axis=AX.X, op=ALU.max)
            p.op("dve", nc.vector.memset, [], [MP], ap=MP[:], constant=0.0)
            for q_ in range(2):
                for dr, order in ((0, ord_f), (1, ord_b)):
                    rs_ = slice(q_ * 64 + dr * 32, q_ * 64 + dr * 32 + 32)
                    for j, c in enumerate(order):
                        p.op("dve", nc.vector.tensor_tensor, [amax, MP], [mc], out=mc[rs_, c:c + 1], in0=amax[rs_, c:c + 1], in1=MP[rs_, c:c + 1], op=ALU.max)
                        if j + 1 < len(order):
                            c2 = order[j + 1]
                            p.op("dve", nc.vector.tensor_tensor, [mc, tot], [MP], out=MP[rs_, c2:c2 + 1], in0=mc[rs_, c:c + 1], in1=tot[rs_, c:c + 1], op=ALU.subtract)
            p.op("dve", nc.vector.tensor_tensor, [MP, mc], [dec], out=dec[:], in0=MP[0:64, :], in1=mc[0:64, :], op=ALU.subtract)
            p.act(dec, dec[:], dec, dec[:], AF.Exp)
            mcb = mc[:].rearrange("p (c o) -> p c o", o=1).broadcast_to([128, NCH, LM])
            p.op("dve", nc.vector.tensor_tensor, [Aa, mc], [Aa], out=Aa[:].rearrange("p (c l) -> p c l", l=LM), in0=Aa[:].rearrange("p (c l) -> p c l", l=LM), in1=mcb, op=ALU.subtract)
            p.op("dve", nc.vector.tensor_tensor, [NBq, mc], [NBq], out=NBq[:].rearrange("p (c l) -> p c l", l=LM), in0=NBq[:].rearrange("p (c l) -> p c l", l=LM), in1=mcb, op=ALU.subtract)
            p.act(WTH, WTH[0:64, :], Aa, Aa[0:64, :], AF.Exp)
            p.act(WTH, WTH[64:128, :], NBq, NBq[64:128, :], AF.Exp)
            for blk in range(NBLK):
                tb = tbank()
                p.mm(tb, [WTH, cstb], transpose=True, out=tb[:, 0:128], in_=WTH[:, blk * 128:(blk + 1) * 128], identity=ident_b)
                p.op("dve", nc.vector.tensor_copy, [tb], [GT], out=GT[:, blk, :], in_=tb[:, 0:128])
            p.op("dve", nc.vector.tensor_tensor, [cst, dec], [dbig], out=dbig[:],
                 in0=cst[0:64, 0, 0:64].rearrange("p (r o) -> p r o", o=1).broadcast_to([64, 64, NCH]),
                 in1=dec[:].rearrange("p (o c) -> p o c", o=1).broadcast_to([64, 64, NCH]), op=ALU.mult)
            rper = 512 // NCH
            r0_ = 0
            while r0_ < 64:
                nr = min(rper, 64 - r0_)
                bk = bank()
                p.mm(bk, [ones64, dbig], out=bk[:, 0:nr * NCH], lhsT=ones64[:, :], rhs=dbig[:, r0_:r0_ + nr, :], start=True, stop=True)
                p.op("dve", nc.vector.tensor_copy, [bk], [decb], out=decb[:, r0_:r0_ + nr, :], in_=bk[:, 0:nr * NCH].rearrange("p (r c) -> p r c", c=NCH))
                r0_ += nr
            p.barrier()
        for b in range(NB):
          with ExitStack() as xs:
            xl = p.sb("xl", [128, 8, TA], BF16, xs)
            p.dma("sp", xl, xl[:], xl_s, xl_s[b, :, :].rearrange("p (k t) -> p k t", t=TA))
            with ExitStack() as m2:
                qT = p.sb("qT", [64, TA], BF16, m2)
                kT = p.sb("kT", [64, TA], BF16, m2)
                kv = p.sb("kv", [128, NBLK, 192], BF16, m2)
                sigo = p.sb("sigo", [128, NBLK_L, 128], BF16, m2)
                Vp = [p.sb("Vp%d" % i, [128, NBLK, 130], BF16, m2) for i in range(2)]
                Hr = [p.sb("Hr%d" % i, [128, NBLK_L, 130], F32, m2) for i in range(2)]
                Hm = p.sb("Hm", [128, NBLK_L, 128], F32, m2)
                dnn = p.sb("dnn", [128, 2, NBLK_L], F32, m2)
                hmb = p.sb("hmb", [128, NBLK_L, 128], BF16, m2)
                hmTh = p.sb("hmTh", [128, T], BF16, m2)
                ssH = p.sb("ssH", [128, NBLK_L], F32, m2)
                ATm = [p.sb("ATm%d" % i, [128, 128], BF16, m2) for i in range(4)]
                Cf = [p.sb("Cf%d" % i, [64, 130], F32, m2) for i in range(2)]
                Cd = [p.sb("Cd%d" % i, [64, 130], BF16, m2) for i in range(4)]
                wqk = [p.sb("wqk%d" % i, [128, 8, 256], BF16, m2) for i in range(2)]
                wtm = [p.sb("wtm%d" % i, [128, 8, 320], BF16, m2) for i in range(2)]
                btm = [p.sb("btm%d" % i, [1, 320], BF16, m2) for i in range(2)]
                ci = 0
                for h in range(8 if STAGE >= 2 else 0):
                    hp, hh = h // 2, h % 2
                    wq = wqk[hp % 2]
                    if hh == 0:
                        p.dma("pool", wq, wq[:], wmfm_d, wmfm_d[hp, :, :].rearrange("p (k c) -> p k c", c=256))
                    wt = wtm[h % 2]
                    bt = btm[h % 2]
                    p.dma("pool", wt, wt[:], wmtm_d, wmtm_d[h, :, :].rearrange("p (k c) -> p k c", c=320))
                    p.dma("pool", bt, bt[:], bmtm_d, bmtm_d[h, :, :])
                    for (t0, n) in ttiles:
                        bq = bank()
                        for k in range(8):
                            p.mm(bq, [wq, xl], last=(k == 7), out=bq[0:64, 0:n], lhsT=wq[:, k, hh * 64:hh * 64 + 64], rhs=xl[:, k, t0:t0 + n], start=(k == 0), stop=(k == 7))
                        bk_ = bank()
                        for k in range(8):
                            p.mm(bk_, [wq, xl], last=(k == 7), out=bk_[0:64, 0:n], lhsT=wq[:, k, 128 + hh * 64:128 + hh * 64 + 64], rhs=xl[:, k, t0:t0 + n], start=(k == 0), stop=(k == 7))
                        vq = V_BMFM + hp * 2
                        p.op("dve", nc.vector.tensor_scalar, [bq, vecs], [qT], out=qT[:, t0:t0 + n], in0=bq[0:64, 0:n],
                             scalar1=vecs[hh * 64:hh * 64 + 64, vq:vq + 1], scalar2=0.125, op0=ALU.add, op1=ALU.mult)
                        p.act(kT, kT[:, t0:t0 + n], bk_, bk_[0:64, 0:n], AF.Identity, extra_reads=[vecs], bias=vecs[hh * 64:hh * 64 + 64, vq + 1:vq + 2], scale=1.0)
                    for blk in range(NBLK):
                        bo = bank()
                        for k in range(8):
                            p.mm(bo, [wt, xl], last=False, out=bo[:, 0:320], lhsT=xl[:, k, blk * 128:(blk + 1) * 128], rhs=wt[:, k, :], start=(k == 0), stop=False)
                        p.mm(bo, [ones1, bt], out=bo[:, 0:320], lhsT=ones1[0:1, :], rhs=bt[0:1, :], start=False, stop=True)
                        p.act(kv, kv[:, blk, :], bo, bo[:, 0:192], AF.Copy)
                        if blk >= NBLK_C:
                            p.act(sigo, sigo[:, blk - NBLK_C, :], bo, bo[:, 192:320], AF.Sigmoid)
                    for dr in range(2):
                        r = dr * 32 + b * 8 + h
                        wcol = GT[:, :, r:r + 1]
                        p.op("dve", nc.vector.tensor_tensor, [kv, GT], [Vp[dr]], out=Vp[dr][:, :, 0:128], in0=kv[:, :, 64:192], in1=wcol.broadcast_to([128, NBLK, 128]), op=ALU.mult)
                        p.op("dve", nc.vector.tensor_copy, [GT], [Vp[dr]], out=Vp[dr][:, :, 128:129], in_=wcol)
                        p.op("dve", nc.vector.memset, [], [Cf[dr]], ap=Cf[dr][:], constant=0.0)
                    for j in range(NBLK):
                        fr = []
                        for dr, order in ((0, ord_f), (1, ord_b)):
                            c = order[j]
                            r = dr * 32 + b * 8 + h
                            dcol = decb[0:64, r, c:c + 1]
                            tk = slice(c * 128, (c + 1) * 128)
                            ci += 1
                            bo = None
                            if c >= NBLK_C:
                                cd = Cd[ci % 4]
                                at = ATm[ci % 4]
                                p.op("dve", nc.vector.tensor_scalar, [Cf[dr], decb], [cd], out=cd[:, 0:129], in0=Cf[dr][:, 0:129], scalar1=dcol, scalar2=None, op0=ALU.mult)
                                bs_ = bank()
                                p.mm(bs_, [kT, qT], out=bs_[:, 0:128], lhsT=kT[:, tk], rhs=qT[:, tk], start=True, stop=True)
                                p.op("dve", nc.vector.tensor_tensor, [bs_, cst], [at], out=at[:], in0=bs_[:, 0:128], in1=cst[:, 2 + dr, :], op=ALU.mult)
                                bo = bank()
                                p.mm(bo, [at, Vp[dr]], last=False, out=bo[:, 0:129], lhsT=at[:], rhs=Vp[dr][:, c, 0:129], start=True, stop=False)
                                p.mm(bo, [qT, cd], out=bo[:, 0:129], lhsT=qT[:, tk], rhs=cd[:, 0:129], start=False, stop=True)
                            bc_ = bank()
                            p.mm(bc_, [kv, Vp[dr]], out=bc_[0:64, 0:129], lhsT=kv[:, c, 0:64], rhs=Vp[dr][:, c, 0:129], start=True, stop=True)
                            fr.append((dr, c, dcol, bo, bc_))
                        for (dr, c, dcol, bo, bc_) in fr:
                            p.op("dve", nc.vector.scalar_tensor_tensor, [Cf[dr], decb, bc_], [Cf[dr]], out=Cf[dr][:, 0:129], in0=Cf[dr][:, 0:129], scalar=dcol,
                                 in1=bc_[0:64, 0:129], op0=ALU.mult, op1=ALU.add)
                            if c >= NBLK_C:
                                p.act(Hr[dr], Hr[dr][:, c - NBLK_C, 0:129], bo, bo[:, 0:129], AF.Copy)
                    for dr in range(2):
                        r = dr * 32 + b * 8 + h
                        dv_ = dnn[:, dr, :].rearrange("p (c o) -> p c o", o=1)
                        p.act(dnn, dv_, Hr[dr], Hr[dr][:, :, 128:129], AF.Abs)
                        p.op("dve", nc.vector.tensor_tensor, [dnn, GT], [dnn], out=dv_, in0=dv_, in1=GT[:, NBLK_C:NBLK, 64 + r:64 + r + 1], op=ALU.max)
                        p.op("dve", nc.vector.reciprocal, [dnn], [dnn], out=dnn[:, dr, :], in_=dnn[:, dr, :])
                        rb_ = dv_.broadcast_to([128, NBLK_L, 128])
                        if dr == 0:
                            p.op("dve", nc.vector.tensor_tensor, [Hr[0], dnn], [Hm], out=Hm[:], in0=Hr[0][:, :, 0:128], in1=rb_, op=ALU.mult)
                        else:
                            p.tt("pool", Hr[1], Hr[1][:, :, 0:128], Hr[1], Hr[1][:, :, 0:128], dnn, rb_, ALU.mult)
                            p.tt("pool", Hm, Hm[:], Hm, Hm[:], Hr[1], Hr[1][:, :, 0:128], ALU.add)
                    tH = Buf(Hr[0].t[:, :, 0:128], "tHv")
                    p.inherit(tH, [Hr[0]])
                    p.tt("pool", tH, tH[:], Hm, Hm[:], Hm, Hm[:], ALU.mult)
                    p.op("dve", nc.vector.tensor_reduce, [tH], [ssH], out=ssH[:], in_=tH[:], axis=AX.X, op=ALU.add)
                    p.act(ssH, ssH[:], ssH, ssH[:], AF.Sqrt, scale=1.0 / 128, bias=EPS)
                    p.op("dve", nc.vector.reciprocal, [ssH], [ssH], out=ssH[:], in_=ssH[:])
                    p.op("dve", nc.vector.tensor_tensor, [Hm, ssH], [tH], out=tH[:], in0=Hm[:], in1=ssH[:].rearrange("p (c o) -> p c o", o=1).broadcast_to([128, NBLK_L, 128]), op=ALU.mult)
                    p.tt("pool", tH, tH[:], tH, tH[:], mgb, mgb[:, h * 128:(h + 1) * 128].rearrange("p (o c) -> p o c", o=1).broadcast_to([128, NBLK_L, 128]), ALU.mult)
                    p.op("dve", nc.vector.tensor_tensor, [tH, sigo], [hmb], out=hmb[:], in0=tH[:], in1=sigo[:], op=ALU.mult)
                    p.inherit(Hr[0], [tH])
                    for c0 in range(0, NBLK_L, 8):
                        n8 = min(8, NBLK_L - c0)
                        tb = tbank()
                        for cl in range(c0, c0 + n8):
                            p.mm(tb, [hmb, cstb], last=(cl == c0 + n8 - 1), transpose=True, out=tb[:, (cl - c0) * 128:(cl - c0 + 1) * 128], in_=hmb[:, cl, :], identity=ident_b)
                        p.act(hmTh, hmTh[:, c0 * 128:(c0 + n8) * 128], tb, tb[:, 0:n8 * 128], AF.Copy)
                    p.dma("sp", hm_s, hm_s[b, :, h * T:(h + 1) * T], hmTh, hmTh[:])
                p.barrier()
            for k in range(8):
                e_ = "pool" if k % 2 else "dve"
                p.op(e_, p.eng_of(e_).tensor_copy, [xl], [xh], out=xh[:, k, 0:CT], in_=xl[:, k, 0:CT])
                p.op(e_, p.eng_of(e_).tensor_copy, [xl], [xh], out=xh[:, k, CT:TA].rearrange("p (c r) -> p c r", r=ROWS),
                     in_=xl[:, k, CT:TA].rearrange("p (r c) -> p c r", c=GW))
            p.barrier()
          if True:
            with ExitStack() as h2:
                vtm = p.sb("vtm", [128, NBLK, 512], BF16, h2)
                qs = p.sb("qs", [128, TA], BF16, h2)
                gsl = p.sb("gsl", [128, T], BF16, h2)
                SG = [p.sb("SG%d" % i, [128, TA], F32, h2) for i in range(2)]
                Gt = p.sb("Gt", [128, TA], F32, h2)
                kks = [p.sb("kk%d" % i, [128, TA], BF16, h2) for i in range(2)]
                qds = [p.sb("qd%d" % i, [128, TA], BF16, h2) for i in range(2)]
                kds = [p.sb("kd%d" % i, [128, TA], BF16, h2) for i in range(2)]
                hmk = p.sb("hmk", [128, TA], BF16, h2)
                totc = p.sb("totc", [128, NCHH], F32, h2)
                dSs = [p.sb("dS%d" % i, [128, NCHH], F32, h2) for i in range(2)]
                Hd = [p.sb("Hd%d" % i, [128, T], F32, h2) for i in range(2)]
                HhT = Hd[0]
                sqb = qds[0]
                hhTh = kds[0]
                rsb = [p.sb("rsb%d" % i, [128, 512], F32, h2) for i in range(2)]
                khm = [[[p.sb("khm%d_%d_%d" % (d_, i, cc), [128, 128], BF16, h2) for cc in range(4)] for i in range(2)] for d_ in range(2)]
                ATh = [[p.sb("ATh%d_%d" % (d_, i), [128, 128], BF16, h2) for i in range(2)] for d_ in range(2)]
                Sfs = [[p.sb("Sf%d_%d" % (d_, i), [128, 128], F32, h2) for i in range(3)] for d_ in range(2)]
                Sbs = [[p.sb("Sb%d_%d" % (d_, i), [128, 128], BF16, h2) for i in range(4)] for d_ in range(2)]
                whf = [p.sb("whf%d" % i, [128, 8, 512], BF16, h2) for i in range(1)]
                wht = p.sb("wht", [128, 8, 512], BF16, h2)
                bht = p.sb("bht", [1, 512], BF16, h2)
                p.dma("pool", hmk, hmk[:], hmask_d, hmask_d[:, :])
                for g in range(2 if STAGE >= 3 else 0):
                    p.dma("pool", wht, wht[:], whtm_d, whtm_d[g, :, :].rearrange("p (k c) -> p k c", c=512))
                    p.dma("pool", bht, bht[:], bhtm_d, bhtm_d[g, :, :])
                    for blk in range(NBLK):
                        bo = bank()
                        for k in range(8):
                            p.mm(bo, [wht, xh], last=False, out=bo[:, :], lhsT=xh[:, k, blk * 128:(blk + 1) * 128], rhs=wht[:, k, :], start=(k == 0), stop=False)
                        p.mm(bo, [ones1, bht], out=bo[:, :], lhsT=ones1[0:1, :], rhs=bht[0:1, :], start=False, stop=True)
                        if blk % 2:
                            p.act(vtm, vtm[:, blk, :], bo, bo[:, :], AF.Copy)
                        else:
                            p.op("dve", nc.vector.tensor_copy, [bo], [vtm], out=vtm[:, blk, :], in_=bo[:, :])
                    for hl in range(4):
                        h = g * 4 + hl
                        wf = whf[0]
                        p.dma("pool", wf, wf[:], whfm_d, whfm_d[h, :, :].rearrange("p (k c) -> p k c", c=512))
                        vb = V_BHFM + h * 4
                        for (t0, n) in ttiles:
                            for part in range(4):
                                if part == 3 and t0 < CT:
                                    continue
                                bk = bank()
                                for k in range(8):
                                    p.mm(bk, [wf, xh], last=(k == 7), out=bk[:, 0:n], lhsT=wf[:, k, part * 128:(part + 1) * 128], rhs=xh[:, k, t0:t0 + n], start=(k == 0), stop=(k == 7))
                                bias = vecs[:, vb + part:vb + part + 1]
                                if part == 0:
                                    p.act(qs, qs[:, t0:t0 + n], bk, bk[:, 0:n], AF.Silu, extra_reads=[vecs], bias=bias, scale=1.0)
                                elif part == 3:
                                    p.act(gsl, gsl[:, t0 - CT:t0 - CT + n], bk, bk[:, 0:n], AF.Silu, extra_reads=[vecs], bias=bias, scale=1.0)
                                else:
                                    sg_ = SG[part - 1]
                                    p.act(sg_, sg_[:, t0:t0 + n], bk, bk[:, 0:n], AF.Sigmoid, extra_reads=[vecs], bias=bias, scale=1.0)
                        for dr in range(2):
                            sg_ = SG[dr]
                            kk, qd, kd, dS = kks[dr], qds[dr], kds[dr], dSs[dr]
                            lcol = dr * 8 + h
                            p.op("pool", nc.gpsimd.tensor_scalar, [sg_, NOML, OML], [kk], out=kk[:], in0=sg_[:], scalar1=NOML[:, lcol:lcol + 1], scalar2=OML[:, lcol:lcol + 1], op0=ALU.mult, op1=ALU.add)
                            p.op("pool", nc.gpsimd.tensor_scalar, [sg_, OML, LB], [sg_], out=sg_[:], in0=sg_[:], scalar1=OML[:, lcol:lcol + 1], scalar2=LB[:, lcol:lcol + 1], op0=ALU.mult, op1=ALU.add)
                            p.act(sg_, sg_[:], sg_, sg_[:], AF.Ln)
                            p.op("dve", nc.vector.tensor_tensor_scan, [hmk, sg_], [Gt], out=Gt[:], data0=hmk[:], data1=sg_[:], initial=0.0, op0=ALU.mult, op1=ALU.add)
                            Gv = Gt[:].rearrange("p (c l) -> p c l", l=LH)
                            tv = totc[:].rearrange("p (c o) -> p c o", o=1)
                            p.op("dve", nc.vector.tensor_copy, [Gt], [totc], out=tv, in_=Gv[:, :, LH - 1:LH])
                            if dr == 1:
                                p.op("dve", nc.vector.tensor_tensor, [sg_, Gt], [Gt], out=Gt[:], in0=sg_[:], in1=Gt[:], op=ALU.subtract)
                                p.op("dve", nc.vector.tensor_tensor, [Gt, totc], [Gt], out=Gv, in0=Gv, in1=tv.broadcast_to([128, NCHH, LH]), op=ALU.add)
                            p.act(dS, dS[:], totc, totc[:], AF.Exp)
                            p.act(sg_, sg_[:], Gt, Gt[:], AF.Exp)
                            p.tt("pool", qd, qd[:], qs, qs[:], sg_, sg_[:], ALU.mult)
                            p.act(sg_, sg_[:], Gt, Gt[:], AF.Exp, scale=-1.0)
                            p.tt("pool", kd, kd[:], kk, kk[:], sg_, sg_[:], ALU.mult)
                            p.op("dve", nc.vector.tensor_tensor, [totc, Gt], [sg_], out=sg_[:].rearrange("p (c l) -> p c l", l=LH), in0=tv.broadcast_to([128, NCHH, LH]), in1=Gv, op=ALU.subtract)
                            p.act(sg_, sg_[:], sg_, sg_[:], AF.Exp)
                            p.tt("pool", kk, kk[:], kk, kk[:], sg_, sg_[:], ALU.mult)
                            p.op("dve", nc.vector.memset, [], [Sfs[dr][0]], ap=Sfs[dr][0][:], constant=0.0)
                            p.op("dve", nc.vector.memset, [], [Sbs[dr][0]], ap=Sbs[dr][0][:], constant=0.0)
                        sis = [0, 0]

                        def hg_front(dr, bi):
                            kk, qd, kd = kks[dr], qds[dr], kds[dr]
                            blk = (ord_f if dr == 0 else ord_b)[bi]
                            tk = slice(blk * 128, (blk + 1) * 128)
                            lat = blk >= NBLK_C
                            bx = bank()
                            p.mm(bx, [kk, cstb], out=bx[:, 0:128], lhsT=kk[:, tk], rhs=ident_b, start=True, stop=True)
                            km = khm[dr][bi % 2]
                            for cc in range(4):
                                mcol = cst[:, 4, cc * 32 + 31:cc * 32 + 32]
                                if HG_ACT_EVAC and cc < 3:
                                    p.act(km[cc], km[cc][:], bx, bx[:, 0:128], AF.Copy, extra_reads=[cst], scale=mcol)
                                else:
                                    p.op("dve", nc.vector.tensor_scalar, [bx, cst], [km[cc]], out=km[cc][:], in0=bx[:, 0:128], scalar1=mcol, scalar2=None, op0=ALU.mult)
                            bu = bank()
                            for cc in range(4):
                                p.mm(bu, [km[cc], vtm], last=(cc == 3), out=bu[:, cc * 128:(cc + 1) * 128], lhsT=km[cc][:], rhs=vtm[:, blk, hl * 128:(hl + 1) * 128], start=True, stop=True)
                            if lat:
                                p.mm(bx, [kd, qd], out=bx[:, 128:256], lhsT=kd[:, tk], rhs=qd[:, tk], start=True, stop=True)
                                at = ATh[dr][bi % 2]
                                p.op("dve", nc.vector.tensor_tensor, [bx, cst], [at], out=at[:], in0=bx[:, 128:256], in1=cst[:, 4 + dr, :], op=ALU.mult)
                            return (dr, bi, blk, lat, bx, bu)

                        def hg_back(ctx_):
                            dr, bi, blk, lat, bx, bu = ctx_
                            qd, dS = qds[dr], dSs[dr]
                            Sf, Sb = Sfs[dr], Sbs[dr]
                            corder = (0, 1, 2, 3) if dr == 0 else (3, 2, 1, 0)
                            if lat:
                                at = ATh[dr][bi % 2]
                                p.mm(bx, [vtm, at], last=False, out=bx[:, 256:384], lhsT=vtm[:, blk, hl * 128:(hl + 1) * 128], rhs=at[:], start=True, stop=False)
                            for ci_, cc in enumerate(corder):
                                c = blk * 4 + cc
                                si = sis[dr]
                                if lat:
                                    p.mm(bx, [Sb[si % 4], qd], last=(ci_ == 3), out=bx[:, 256 + cc * 32:256 + (cc + 1) * 32], lhsT=Sb[si % 4][:], rhs=qd[:, c * 32:(c + 1) * 32],
                                         start=False, stop=(ci_ == 3))
                                p.op("dve", nc.vector.scalar_tensor_tensor, [Sf[si % 3], dS, bu], [Sf[(si + 1) % 3]], out=Sf[(si + 1) % 3][:], in0=Sf[si % 3][:], scalar=dS[:, c:c + 1],
                                     in1=bu[:, cc * 128:(cc + 1) * 128], op0=ALU.mult, op1=ALU.add)
                                if ci_ % 2 == 0 and HG_POOL_CAST:
                                    p.op("pool", nc.gpsimd.tensor_copy, [Sf[(si + 1) % 3]], [Sb[(si + 1) % 4]], out=Sb[(si + 1) % 4][:], in_=Sf[(si + 1) % 3][:])
                                else:
                                    p.act(Sb[(si + 1) % 4], Sb[(si + 1) % 4][:], Sf[(si + 1) % 3], Sf[(si + 1) % 3][:], AF.Copy)
                                sis[dr] = si + 1
                            if lat:
                                lt = slice((blk - NBLK_C) * 128, (blk - NBLK_C + 1) * 128)
                                p.act(Hd[dr], Hd[dr][:, lt], bx, bx[:, 256:384], AF.Copy)

                        if HG_SEQ == 0:
                            seq_ = [(dr, bi) for bi in range(NBLK) for dr in range(2)]
                        else:
                            seq_ = [(dr, bi) for dr in range(2) for bi in range(NBLK)]
                        if HG_PIPE:
                            pend = hg_front(*seq_[0])
                            for qi in range(len(seq_)):
                                nxt = hg_front(*seq_[qi + 1]) if qi + 1 < len(seq_) else None
                                hg_back(pend)
                                pend = nxt
                        else:
                            for qi in range(len(seq_)):
                                hg_back(hg_front(*seq_[qi]))
                        p.tt("pool", Hd[0], Hd[0][:], Hd[0], Hd[0][:], Hd[1], Hd[1][:], ALU.add)
                        p.act(sqb, sqb[:, 0:T], HhT, HhT[:], AF.Square)
                        ncol = NTL // ROWS
                        hv = hhTh[:, 0:T].rearrange("p (r c) -> p c r", c=GW)
                        for ti, t0 in enumerate(range(0, T, NTL)):
                            bk = bank()
                            p.mm(bk, [cstb, sqb], out=bk[:, :], lhsT=ones_b, rhs=sqb[:, t0:t0 + NTL], start=True, stop=True)
                            rs_ = rsb[ti % 2]
                            p.act(rs_, rs_[:], bk, bk[:, :], AF.Sqrt, scale=1.0 / 128, bias=EPS)
                            p.op("dve", nc.vector.reciprocal, [rs_], [rs_], out=rs_[:], in_=rs_[:])
                            p.op("dve", nc.vector.scalar_tensor_tensor, [HhT, vecs, rs_], [rs_], out=rs_[:], in0=HhT[:, t0:t0 + NTL], scalar=vecs[:, V_HN + h:V_HN + h + 1], in1=rs_[:], op0=ALU.mult, op1=ALU.mult)
                            c0 = t0 // ROWS
                            p.tt("dve", hhTh, hv[:, c0:c0 + ncol, :], rs_, rs_[:].rearrange("p (c r) -> p c r", r=ROWS), gsl, gsl[:, t0:t0 + NTL].rearrange("p (c r) -> p c r", r=ROWS), ALU.mult)
                        p.dma("sp", hh_s, hh_s[b, :, h * T:(h + 1) * T], hhTh, hhTh[:, 0:T])
                p.barrier()

    with ExitStack() as ph:
        alloc_ffn(ph, "_c")
        bct, xts, xmT, gbf, t2s = FB["bct"], FB["xts"], FB["xmT"], FB["gbf"], FB["t2s"]
        ss, rstd, junk = FB["ss"], FB["rstd"], FB["junk"]
        zbf = Buf(gbf.t[:, 0:8, :], "zbf")
        hmT = Buf(gbf.t[:, 8:16, :], "hmT")
        hhT = p.sb("hhT", [128, 8, NTL], BF16, ph)
        xlT = xmT
        sgs = [p.sb("sg%d" % i, [128, NTL], F32, ph) for i in range(2)]
        z1s = [p.sb("z1_%d" % i, [128, NTL], F32, ph) for i in range(2)]
        wmgr = [p.sb("wmgr%d" % i, [128, 8, 256], BF16, ph) for i in range(2)]
        wpjr = [p.sb("wpjr%d" % i, [128, 8, 256], BF16, ph) for i in range(2)]
        gatem = bct["gs2"]
        fin = bct["sh2"]
        bc_load(fin, rows_d, rows_d[3:4, :])
        tix = 0
        for b in range(NB):
            bc_load(gatem, modd, modd[b:b + 1, 5 * D:6 * D])
            load_mod(b, 6, 7, 8, 2, bct["gs"], bct["sh"], bct["gate"])
            for tok0 in range(0, T, NTL):
                NT = NTL
                J = 4
                tix += 1
                xt = xts[tix % 2]
                r0 = b * T + tok0
                p.dma("sp", xt, xt[:, 0:J, :], x1_s, x1_s[r0:r0 + NT, :].rearrange("(j p) d -> p j d", p=128))
                p.inherit(zbf, [gbf])
                p.inherit(hmT, [gbf])
                p.dma("sp", hmT, hmT[:], hm_s, hm_s[b, :, :].rearrange("p (k t) -> p k t", t=T)[:, :, tok0:tok0 + NT])
                p.dma("sp", hhT, hhT[:], hh_s, hh_s[b, :, :].rearrange("p (k t) -> p k t", t=T)[:, :, tok0:tok0 + NT])
                p.dma("sp", xlT, xlT[:], xl_s, xl_s[b, :, :].rearrange("p (k t) -> p k t", t=TA)[:, :, CT + tok0:CT + tok0 + NT])
                for mc in range(8):
                    wg_ = wmgr[mc % 2]
                    wp_ = wpjr[mc % 2]
                    p.dma("sp", wg_, wg_[:], wmg_b, wmg_b[mc, :, :].rearrange("p (k c) -> p k c", c=256))
                    p.dma("sp", wp_, wp_[:], wpj_b, wpj_b[mc, :, :].rearrange("p (k c) -> p k c", c=256))
                    for which in range(2):
                        src_h = hmT if which == 0 else hhT
                        bg = bank()
                        for k in range(8):
                            p.mm(bg, [wg_, xlT], last=(k == 7), out=bg[:, :], lhsT=wg_[:, k, which * 128:(which + 1) * 128], rhs=xlT[:, k, :],
                                 start=(k == 0), stop=(k == 7))
                        by = bank()
                        for k in range(8):
                            p.mm(by, [wp_, src_h], last=(k == 7), out=by[:, :], lhsT=wp_[:, k, which * 128:(which + 1) * 128], rhs=src_h[:, k, :],
                                 start=(k == 0), stop=(k == 7))
                        sg = sgs[which]
                        vcol = V_BMG + mc * 2 + which
                        p.act(sg, sg[:], bg, bg[:, :], AF.Sigmoid, extra_reads=[vecs], bias=vecs[:, vcol:vcol + 1], scale=1.0)
                        z1 = z1s[mc % 2]
                        if which == 0:
                            p.op("dve", nc.vector.tensor_tensor, [sg, by], [z1], out=z1[:], in0=sg[:], in1=by[:, :], op=ALU.mult)
                        else:
                            p.op("dve", nc.vector.tensor_tensor, [sg, by], [sg], out=sg[:], in0=sg[:], in1=by[:, :], op=ALU.mult)
                            p.tt("pool", zbf, zbf[:, mc, :], sg, sg[:], z1, z1[:], ALU.add)
                for nh in range(2):
                    cnt["wo"] += 1
                    wo = FB["wor"][cnt["wo"] % 3]
                    p.dma("sp", wo, wo[:, 0:8, :], wmo_b, wmo_b[nh, :, :].rearrange("p (k c) -> p k c", c=512))
                    for j in range(J):
                        bo = bank()
                        for k in range(8):
                            p.mm(bo, [zbf, wo], last=(k == 7), out=bo[:, :], lhsT=zbf[:, k, j * 128:(j + 1) * 128], rhs=wo[:, k, :],
                                 start=(k == 0), stop=(k == 7))
                        cnt["t2"] += 1
                        t2 = t2s[cnt["t2"] % 2]
                        p.op("dve", nc.vector.tensor_tensor, [bo, gatem], [t2], out=t2[:], in0=bo[:, :], in1=gatem[:, nh * 512:(nh + 1) * 512], op=ALU.mult)
                        p.tt("pool", xt, xt[:, j, nh * 512:(nh + 1) * 512], xt, xt[:, j, nh * 512:(nh + 1) * 512], t2, t2[:], ALU.add)
                p.inherit(gbf, [zbf, hmT])
                norm_mod_T(xt, J, bct["gs"], bct["sh"])
                ffn(xt, J, bct["gate"], w2i_b, w2o_b)
                p.op("dve", nc.vector.memset, [], [ss], ap=ss[:], constant=0.0)
                for j in range(J):
                    p.act(junk, junk[:], xt, xt[:, j, :], AF.Square, extra_reads=[ss], accum_out=ss[:, j:j + 1])
                    ss.w = [("act", p.cnt["act"])]
                p.act(rstd, rstd[:, 0:J], ss, ss[:, 0:J], AF.Sqrt, scale=1.0 / D, bias=EPS)
                p.op("dve", nc.vector.reciprocal, [rstd], [rstd], out=rstd[:, 0:J], in_=rstd[:, 0:J])
                for j in range(J):
                    p.op("dve", nc.vector.scalar_tensor_tensor, [xt, rstd, fin], [xt], out=xt[:, j, :], in0=xt[:, j, :],
                         scalar=rstd[:, j:j + 1], in1=fin[:], op0=ALU.mult, op1=ALU.mult)
                p.dma("sp", out_d, out_d[r0:r0 + NT, :].rearrange("(j p) d -> p j d", p=128), xt, xt[:, 0:J, :])
    p.barrier()
    return nc, p


def _tile_k(W):
    C = W.shape[1]
    return np.ascontiguousarray(W.reshape(8, 128, C).transpose(1, 0, 2)).reshape(128, 8 * C)


def _tile_w_in(W):
    a = W[:, :FH].reshape(8, 128, NI, 128)
    u = W[:, FH:].reshape(8, 128, NI, 128)
    t = np.concatenate([a, u], axis=3)
    return np.ascontiguousarray(t.transpose(2, 1, 0, 3)).reshape(NI, 128, 2048)


def _tile_w_out(W):
    t = W.reshape(NI, 128, 2, 512)
    return np.ascontiguousarray(t.transpose(2, 1, 0, 3)).reshape(2, 128, NI * 512)


def prep_shared(inp, T, CT):
    f = lambda a: np.asarray(a, dtype=np.float32)
    TA = CT + T
    W = f(inp["mix_w_in"])[0]
    bI = f(inp["mix_b_in"])[0]
    sh = {}
    sh["ada_w"] = np.ascontiguousarray(f(inp["ada_w"])[0])
    sh["ada_b"] = np.ascontiguousarray(f(inp["ada_b"])[0][None, :])
    sh["rows"] = np.stack([f(inp["ffn1_norm"])[0], f(inp["mix_norm"])[0], f(inp["ffn2_norm"])[0], f(inp["final_norm"]), f(inp["mlstm_norm"])[0]])
    vec = np.zeros((128, NVEC), np.float32)
    vec[0:16, V_BG] = bI[O_IG:O_IG + 16]
    vec[16:32, V_BG] = bI[O_FG:O_FG + 16]
    for hp in range(4):
        vec[:, V_BMFM + hp * 2 + 0] = bI[O_MQ + hp * 128:O_MQ + (hp + 1) * 128]
        vec[:, V_BMFM + hp * 2 + 1] = bI[O_MK + hp * 128:O_MK + (hp + 1) * 128]
    for h in range(8):
        vec[:, V_BHFM + h * 4 + 0] = bI[O_HQ + h * 128:O_HQ + (h + 1) * 128]
        vec[:, V_BHFM + h * 4 + 1] = bI[O_HF + h * 128:O_HF + (h + 1) * 128]
        vec[:, V_BHFM + h * 4 + 2] = bI[O_HF + 1024 + h * 128:O_HF + 1024 + (h + 1) * 128]
        vec[:, V_BHFM + h * 4 + 3] = bI[O_HG + h * 128:O_HG + (h + 1) * 128]
        vec[:, V_HN + h] = f(inp["hgrn_norm"])[0][h * 128:(h + 1) * 128]
    for mc in range(8):
        vec[:, V_BMG + mc * 2 + 0] = bI[O_GM + mc * 128:O_GM + (mc + 1) * 128]
        vec[:, V_BMG + mc * 2 + 1] = bI[O_GH + mc * 128:O_GH + (mc + 1) * 128]
    lbl = f(inp["hgrn_lb_logits"])
    for d_ in range(2):
        for l_ in range(2):
            for h in range(8):
                vec[:, V_LB + d_ * 16 + l_ * 8 + h] = lbl[d_, l_, h * 128:(h + 1) * 128]
    sh["vecs"] = vec
    cst = np.zeros((128, 6, 128), np.float32)
    cst[:, 0] = np.eye(128)
    cst[:, 1] = 1.0
    s_ = np.arange(128)[:, None]
    t_ = np.arange(128)[None, :]
    cst[:, 2] = (s_ <= t_)
    cst[:, 3] = (s_ >= t_)
    same = (s_ // 32) == (t_ // 32)
    cst[:, 4] = (s_ <= t_) & same
    cst[:, 5] = (s_ >= t_) & same
    sh["consts"] = cst.reshape(128, 6 * 128)
    cm = np.ones((64, TA), np.float32)
    cm[:, ::128] = 0.0
    sh["cmask"] = cm
    hm = np.ones((128, TA), np.float32)
    hm[:, ::32] = 0.0
    sh["hmask"] = hm
    sel = np.zeros((8, 8, 64), np.float32)
    for h in range(8):
        sel[h, h, :] = 1.0
    sh["sel"] = sel.reshape(8, 8 * 64)
    sh["w1i"] = _tile_w_in(f(inp["ffn1_w_in"])[0])
    sh["w1o"] = _tile_w_out(f(inp["ffn1_w_out"])[0])
    sh["w2i"] = _tile_w_in(f(inp["ffn2_w_in"])[0])
    sh["w2o"] = _tile_w_out(f(inp["ffn2_w_out"])[0])
    sh["wg"] = _tile_k(np.concatenate([W[:, O_IG:O_IG + 16], W[:, O_FG:O_FG + 16]], 1))
    sh["wmfm"] = np.stack([_tile_k(np.concatenate([W[:, O_MQ + hp * 128:O_MQ + (hp + 1) * 128], W[:, O_MK + hp * 128:O_MK + (hp + 1) * 128]], 1)) for hp in range(4)])
    sh["wmtm"] = np.stack([_tile_k(np.concatenate([W[:, O_MK + h * 64:O_MK + (h + 1) * 64], W[:, O_MV + h * 128:O_MV + (h + 1) * 128],
                                                   W[:, O_MO + h * 128:O_MO + (h + 1) * 128]], 1)) for h in range(8)])
    sh["bmtm"] = np.stack([np.concatenate([bI[O_MK + h * 64:O_MK + (h + 1) * 64], bI[O_MV + h * 128:O_MV + (h + 1) * 128],
                                           bI[O_MO + h * 128:O_MO + (h + 1) * 128]])[None, :] for h in range(8)])
    sh["whfm"] = np.stack([_tile_k(np.concatenate([W[:, O_HQ + h * 128:O_HQ + (h + 1) * 128], W[:, O_HF + h * 128:O_HF + (h + 1) * 128],
                                                   W[:, O_HF + 1024 + h * 128:O_HF + 1024 + (h + 1) * 128],
                                                   W[:, O_HG + h * 128:O_HG + (h + 1) * 128]], 1)) for h in range(8)])
    sh["whtm"] = np.stack([_tile_k(W[:, O_HI + g * 512:O_HI + (g + 1) * 512]) for g in range(2)])
    sh["bhtm"] = np.stack([bI[O_HI + g * 512:O_HI + (g + 1) * 512][None, :] for g in range(2)])
    pm = f(inp["proj_m"])[0]
    ph_ = f(inp["proj_h"])[0]
    sh["wmg"] = np.stack([_tile_k(np.concatenate([W[:, O_GM + mc * 128:O_GM + (mc + 1) * 128], W[:, O_GH + mc * 128:O_GH + (mc + 1) * 128]], 1)) for mc in range(8)])
    sh["wpj"] = np.stack([_tile_k(np.concatenate([pm[:, mc * 128:(mc + 1) * 128], ph_[:, mc * 128:(mc + 1) * 128]], 1)) for mc in range(8)])
    wo = f(inp["mix_w_out"])[0]
    sh["wmo"] = np.stack([_tile_k(wo[:, nh * 512:(nh + 1) * 512]) for nh in range(2)])
    return {k: np.ascontiguousarray(v, dtype=np.float32) for k, v in sh.items()}


def prep_core(inp, bs):
    f = lambda a: np.asarray(a, dtype=np.float32)
    NB = len(bs)
    x = f(inp["x"])[bs]
    cx = f(inp["ctx"])[bs]
    cmat = np.concatenate([f(inp["c"])[bs], f(inp["c_ctx"])[None, :]], 0)
    cc = np.ascontiguousarray(cmat.reshape(NB + 1, 8, 128).transpose(2, 1, 0)).reshape(128, 8 * (NB + 1))
    return {"x": np.ascontiguousarray(x.reshape(-1, D)), "ctx": np.ascontiguousarray(cx.reshape(-1, D)), "cc": cc}


_CACHE = {}


def kernel(**inputs):
    B, T, _ = inputs["x"].shape
    CT = inputs["ctx"].shape[1]
    NB = B // NCORES
    key = (NB, T, CT)
    if key not in _CACHE:
        _CACHE[key] = build(NB, T, CT)[0]
    nc = _CACHE[key]
    shared = prep_shared(inputs, T, CT)
    in_maps = []
    for c in range(NCORES):
        m = dict(shared)
        m.update(prep_core(inputs, list(range(c * NB, (c + 1) * NB))))
        in_maps.append(m)
    res = run_bass_kernel_spmd(nc, in_maps, core_ids=list(range(NCORES)))
    out = np.concatenate([np.asarray(r["out"]).reshape(NB, T, D) for r in res.results], axis=0)
    return out.astype(np.float32)
```

```python
import numpy as np
import concourse.bass as bass
import concourse.mybir as mybir
from concourse.bass_utils import run_bass_kernel_spmd
from contextlib import ExitStack

F32 = mybir.dt.float32
BF16 = mybir.dt.bfloat16
AF = mybir.ActivationFunctionType
ALU = mybir.AluOpType
AX = mybir.AxisListType

D = 1024
FH = 2816
NI = 22
EPS = 1e-6
NCORES = 8
STAGE = 3
HG_ACT_EVAC = False
HG_PIPE = False
HG_SEQ = 2
HG_POOL_CAST = False
FORCE_LAST = False


class Buf:
    __slots__ = ("t", "w", "r", "name")

    def __init__(self, t, name=""):
        self.t = t
        self.w = []
        self.r = []
        self.name = name

    def __getitem__(self, idx):
        return self.t[idx]


class Prog:
    def __init__(self, nc, n_dma_ch=8):
        self.nc = nc
        self.es = ExitStack()
        self.eng = {"pe": nc.tensor, "act": nc.scalar, "dve": nc.vector, "pool": nc.gpsimd, "sp": nc.sync}
        self.sem = {}
        self.cnt = {}
        for e in ("pe", "act", "dve", "pool"):
            self.sem[e] = self.es.enter_context(nc.semaphore("s_" + e))
            self.cnt[e] = 0
        self.ch = {}
        self.chi = {}
        for q in ("sp", "pool"):
            lst = []
            for i in range(n_dma_ch):
                k = "d_%s%d" % (q, i)
                self.sem[k] = self.es.enter_context(nc.semaphore(k))
                self.cnt[k] = 0
                lst.append(k)
            self.ch[q] = lst
            self.chi[q] = 0
        self.seen = {e: {} for e in self.eng}
        self.n_inst = 0
        self.n_wait = 0
        self.rr = 0

    def sb(self, name, shape, dt, stack=None):
        self.uid = getattr(self, "uid", 0) + 1
        name = "%s_u%d" % (name, self.uid)
        t = (stack or self.es).enter_context(self.nc.sbuf_tensor(name, list(shape), dt))
        return Buf(t, name)

    def ps(self, name, shape, dt, stack=None):
        t = (stack or self.es).enter_context(self.nc.psum_tensor(name, list(shape), dt))
        return Buf(t, name)

    def dram(self, name, shape, dt, kind="Internal"):
        t = self.nc.dram_tensor(name, list(shape), dt, kind=kind)
        return Buf(t.ap(), name)

    def _wait(self, e, deps):
        need = {}
        for (k, v) in deps:
            if v > need.get(k, 0):
                need[k] = v
        seen = self.seen[e]
        for k, v in need.items():
            if k == "pe" and e == "pe":
                continue
            if seen.get(k, 0) >= v:
                continue
            self.eng[e].wait_ge(self.sem[k], v)
            self.n_wait += 1
            seen[k] = v

    def _deps(self, reads, writes):
        deps = []
        for b in reads:
            deps.extend(b.w)
        for b in writes:
            deps.extend(b.w)
            deps.extend(b.r)
        return deps

    def _record(self, tick, reads, writes):
        writes = [b for b in writes if b.name != "junk"]
        for b in reads:
            b.r.append(tick)
            if len(b.r) > 48:
                m = {}
                for (k, v) in b.r:
                    if v > m.get(k, 0):
                        m[k] = v
                b.r = list(m.items())
        for b in writes:
            b.w = [tick]
            b.r = []

    def op(self, e, fn, reads=(), writes=(), **kw):
        self._wait(e, self._deps(reads, writes))
        ins = fn(**kw)
        self.cnt[e] += 1
        ins.then_inc(self.sem[e], 1)
        self._record((e, self.cnt[e]), reads, writes)
        self.n_inst += 1
        return ins

    def mm(self, out_buf, reads, last=True, transpose=False, **kw):
        e = "pe"
        if FORCE_LAST:
            last = True
        self._wait(e, self._deps(reads, [out_buf]))
        if transpose:
            ins = self.nc.tensor.transpose(**kw)
        else:
            ins = self.nc.tensor.matmul(**kw)
        self._record((e, self.cnt[e] + 1), reads, [out_buf])
        self.n_inst += 1
        if last:
            self.cnt[e] += 1
            ins.then_inc(self.sem[e], 1)
        return ins

    def dma(self, q, out_buf, out_ap, in_buf, in_ap, **kw):
        chs = self.ch[q]
        k = chs[self.chi[q] % len(chs)]
        self.chi[q] += 1
        deps = self._deps([in_buf], [out_buf])
        if self.cnt[k] > 0:
            deps.append((k, self.cnt[k]))
        self._wait(q, deps)
        ins = self.eng[q].dma_start(out=out_ap, in_=in_ap, **kw)
        self.cnt[k] += 16
        ins.then_inc(self.sem[k], 16)
        self._record((k, self.cnt[k]), [in_buf], [out_buf])
        self.n_inst += 1
        return ins

    def inherit(self, new, olds):
        for o in olds:
            new.w = list(new.w) + list(o.w)
            new.r = list(new.r) + list(o.r)

    def barrier(self):
        allk = [(k, v) for k, v in self.cnt.items() if v > 0]
        for e in self.eng:
            self._wait(e, allk)

    def ve(self):
        self.rr += 1
        return "dve" if (self.rr % 3) else "pool"

    def eng_of(self, e):
        return self.nc.vector if e == "dve" else self.nc.gpsimd

    def tt(self, e, out_b, out, in0_b, in0, in1_b, in1, op):
        self.op(e, self.eng_of(e).tensor_tensor, [in0_b, in1_b], [out_b], out=out, in0=in0, in1=in1, op=op)

    def act(self, out_b, out, in_b, in_, func, extra_reads=(), **kw):
        self.op("act", self.nc.scalar.activation, [in_b] + list(extra_reads), [out_b], out=out, in_=in_, func=func, **kw)


O_MQ, O_MK, O_MV, O_MO, O_IG, O_FG, O_HQ, O_HF, O_HI, O_HG, O_GM, O_GH = (
    0, 512, 1024, 2048, 3072, 3088, 3104, 4128, 6176, 7200, 8224, 9248)

V_BG = 0
V_BMFM = 1
V_BHFM = 9
V_BMG = 41
V_HN = 57
V_LB = 65
NVEC = 97


def build(NB, T, CT, dbg=False):
    GW = 64
    ROWS = T // GW
    TA = CT + T
    LM = 128
    LH = 32
    NBLK = TA // 128
    NBLK_C = CT // 128
    NCHH = TA // LH
    NTL = 512
    NB1 = NB + 1

    nc = bass.Bass("TRN2", target_bir_lowering=False)
    p = Prog(nc)

    def din(name, shape, dt=F32):
        return Buf(nc.dram_tensor(name, list(shape), dt, kind="ExternalInput").ap(), name)

    x_d = din("x", [NB * T, D])
    ctx_d = din("ctx", [NB * CT, D])
    cc_d = din("cc", [128, 8 * NB1])
    adaw_d = din("ada_w", [D, 9 * D])
    adab_d = din("ada_b", [1, 9 * D])
    rows_d = din("rows", [5, D])
    vecs_d = din("vecs", [128, NVEC])
    consts_d = din("consts", [128, 6 * 128])
    cmask_d = din("cmask", [64, TA])
    hmask_d = din("hmask", [128, TA])
    sel_d = din("sel", [8, 8 * 64])
    w1i_d = din("w1i", [NI, 128, 2048])
    w1o_d = din("w1o", [2, 128, NI * 512])
    w2i_d = din("w2i", [NI, 128, 2048])
    w2o_d = din("w2o", [2, 128, NI * 512])
    wg_d = din("wg", [128, 8 * 32])
    wmfm_d = din("wmfm", [4, 128, 8 * 256])
    wmtm_d = din("wmtm", [8, 128, 8 * 320])
    bmtm_d = din("bmtm", [8, 1, 320])
    whfm_d = din("whfm", [8, 128, 8 * 512])
    whtm_d = din("whtm", [2, 128, 8 * 512])
    bhtm_d = din("bhtm", [2, 1, 512])
    wmg_d = din("wmg", [8, 128, 8 * 256])
    wpj_d = din("wpj", [8, 128, 8 * 256])
    wmo_d = din("wmo", [2, 128, 8 * 512])
    out_d = Buf(nc.dram_tensor("out", [NB * T, D], F32, kind="ExternalOutput").ap(), "out")

    modd = p.dram("modd", [NB1, 9 * D], F32)
    x1_s = p.dram("x1_s", [NB * T, D], F32)
    xl_s = p.dram("xl_s", [NB, 128, 8 * TA], BF16)
    gates_s = p.dram("gates_s", [NB, 32, TA], F32)
    hm_s = p.dram("hm_s", [NB, 128, 8 * T], BF16)
    hh_s = p.dram("hh_s", [NB, 128, 8 * T], BF16)
    dbg_out = {}

    cst = p.sb("cst", [128, 6, 128], F32)
    cstb = p.sb("cstb", [128, 6, 128], BF16)
    vecs = p.sb("vecs_sb", [128, NVEC], F32)
    ones1 = p.sb("ones1", [1, 128], BF16)
    p.dma("sp", cst, cst[:], consts_d, consts_d[:, :].rearrange("p (a b) -> p a b", b=128))
    p.dma("sp", vecs, vecs[:], vecs_d, vecs_d[:, :])
    p.op("dve", nc.vector.tensor_copy, [cst], [cstb], out=cstb[:], in_=cst[:])
    p.op("dve", nc.vector.memset, [], [ones1], ap=ones1[:], constant=1.0)
    ident_b = cstb[:, 0, :]
    ones_b = cstb[:, 1, :]

    banks = [p.ps("bank%d" % i, [128, 512], F32) for i in range(6)]
    tbanks = [p.ps("tbank%d" % i, [128, 1024], BF16) for i in range(2)]
    bstate = {"i": 0, "t": 0}

    def bank():
        bstate["i"] += 1
        return banks[bstate["i"] % len(banks)]

    def tbank():
        bstate["t"] += 1
        return tbanks[bstate["t"] % 2]

    w1i_b = p.dram("w1i_b", [NI, 128, 2048], BF16)
    w1o_b = p.dram("w1o_b", [2, 128, NI * 512], BF16)
    w2i_b = p.dram("w2i_b", [NI, 128, 2048], BF16)
    w2o_b = p.dram("w2o_b", [2, 128, NI * 512], BF16)
    wmg_b = p.dram("wmg_b", [8, 128, 2048], BF16)
    wpj_b = p.dram("wpj_b", [8, 128, 2048], BF16)
    wmo_b = p.dram("wmo_b", [2, 128, 4096], BF16)

    PC = {"gen": None}

    def precast_gen(stk, which):
        stg = [p.sb("stg%d" % i, [128, 2816], BF16, stk) for i in range(4)]
        si_ = [0]

        def one(src, dst, idx, n):
            c0 = 0
            while c0 < n:
                w_ = min(2816, n - c0)
                st = stg[si_[0] % 4]
                si_[0] += 1
                p.dma("pool", st, st[:, 0:w_], src, src[idx, :, c0:c0 + w_])
                p.dma("sp", dst, dst[idx, :, c0:c0 + w_], st, st[:, 0:w_])
                c0 += w_
                yield
        if which == 0:
            for i in range(NI):
                yield from one(w1i_d, w1i_b, i, 2048)
            for nh in range(2):
                yield from one(w1o_d, w1o_b, nh, NI * 512)
        else:
            for mc in range(8):
                yield from one(wmg_d, wmg_b, mc, 2048)
                yield from one(wpj_d, wpj_b, mc, 2048)
            for nh in range(2):
                yield from one(wmo_d, wmo_b, nh, 4096)
            for i in range(NI):
                yield from one(w2i_d, w2i_b, i, 2048)
            for nh in range(2):
                yield from one(w2o_d, w2o_b, nh, NI * 512)

    def pump(n=1):
        g = PC["gen"]
        if g is None:
            return
        for _ in range(n):
            try:
                next(g)
            except StopIteration:
                PC["gen"] = None
                return

    with ExitStack() as ph:
        PC["gen"] = precast_gen(ph, 0)
        pump(1000)
        ccs = p.sb("ccs", [128, 8, NB1], F32, ph)
        scb = p.sb("scb", [128, 8, NB1], BF16, ph)
        p.dma("sp", ccs, ccs[:], cc_d, cc_d[:, :].rearrange("p (k n) -> p k n", n=NB1))
        p.act(scb, scb[:], ccs, ccs[:], AF.Silu)
        awr = [p.sb("awr%d" % i, [128, 8, 512], BF16, ph) for i in range(2)]
        abr = [p.sb("abr%d" % i, [1, 512], BF16, ph) for i in range(2)]
        mrow = [p.sb("mrow%d" % i, [NB1, 512], F32, ph) for i in range(2)]
        adaw_v = adaw_d[:, :].rearrange("(k p) n -> p k n", p=128)
        for n in range(18):
            aw = awr[n % 2]
            ab = abr[n % 2]
            mr = mrow[n % 2]
            p.dma("pool", aw, aw[:], adaw_d, adaw_v[:, :, n * 512:(n + 1) * 512])
            p.dma("pool", ab, ab[:], adab_d, adab_d[:, n * 512:(n + 1) * 512])
            bk = bank()
            for k in range(8):
                p.mm(bk, [scb, aw], last=False, out=bk[0:NB1, :], lhsT=scb[:, k, :], rhs=aw[:, k, :], start=(k == 0), stop=False)
            p.mm(bk, [ones1, ab], out=bk[0:NB1, :], lhsT=ones1[0:1, 0:NB1], rhs=ab[0:1, :], start=False, stop=True)
            p.op("dve", nc.vector.tensor_copy, [bk], [mr], out=mr[:], in_=bk[0:NB1, :])
            p.dma("sp", modd, modd[:, n * 512:(n + 1) * 512], mr, mr[:])
        p.barrier()

    FB = {}
    cnt = {"wi": 0, "wo": 0, "tmp": 0, "sil": 0, "t2": 0}

    def alloc_ffn(ffs, tag):
        FB["xts"] = [p.sb("xt%d%s" % (i, tag), [128, 4, D], F32, ffs) for i in range(2)]
        FB["junk"] = p.sb("junk" + tag, [128, D], BF16, ffs)
        FB["junk"].name = "junk"
        FB["tmps"] = [p.sb("tmp%d%s" % (i, tag), [128, D], F32, ffs) for i in range(2)]
        FB["xm"] = p.sb("xm" + tag, [128, 4, D], BF16, ffs)
        FB["xmT"] = p.sb("xmT" + tag, [128, 8, NTL], BF16, ffs)
        FB["gbf"] = p.sb("gbf" + tag, [128, NI, NTL], BF16, ffs)
        FB["sils"] = [p.sb("sil%d%s" % (i, tag), [128, NTL], F32, ffs) for i in range(2)]
        FB["t2s"] = [p.sb("t2_%d%s" % (i, tag), [128, 512], F32, ffs) for i in range(2)]
        FB["wir"] = [p.sb("wir%d%s" % (i, tag), [128, 8, 256], BF16, ffs) for i in range(4)]
        FB["wor"] = [p.sb("wor%d%s" % (i, tag), [128, 11, 512], BF16, ffs) for i in range(3)]
        FB["ss"] = p.sb("ss" + tag, [128, 8], F32, ffs)
        FB["rstd"] = p.sb("rstd" + tag, [128, 8], F32, ffs)
        FB["bct"] = {k: p.sb("bc_" + k + tag, [128, D], F32, ffs) for k in ("gs", "sh", "gate", "gs2", "sh2", "nrm")}

    def bc_load(dst, src_buf, src_ap):
        p.dma("sp", dst, dst[:], src_buf, src_ap.partition_broadcast(128))

    def load_mod(b, i_shift, i_scale, i_gate, nrm_row, gs, sh, gate):
        bc_load(FB["bct"]["nrm"], rows_d, rows_d[nrm_row:nrm_row + 1, :])
        bc_load(gs, modd, modd[b:b + 1, i_scale * D:(i_scale + 1) * D])
        bc_load(sh, modd, modd[b:b + 1, i_shift * D:(i_shift + 1) * D])
        if gate is not None:
            bc_load(gate, modd, modd[b:b + 1, i_gate * D:(i_gate + 1) * D])
        p.op("dve", nc.vector.scalar_tensor_tensor, [gs, FB["bct"]["nrm"]], [gs], out=gs[:], in0=gs[:], scalar=1.0,
             in1=FB["bct"]["nrm"][:], op0=ALU.add, op1=ALU.mult)

    def norm_mod_T(xt, J, gs, sh):
        ss, rstd, junk, tmps, xm, xmT = FB["ss"], FB["rstd"], FB["junk"], FB["tmps"], FB["xm"], FB["xmT"]
        p.op("dve", nc.vector.memset, [], [ss], ap=ss[:], constant=0.0)
        for j in range(J):
            p.act(junk, junk[:], xt, xt[:, j, :], AF.Square, extra_reads=[ss], accum_out=ss[:, j:j + 1])
            ss.w = [("act", p.cnt["act"])]
        p.act(rstd, rstd[:, 0:J], ss, ss[:, 0:J], AF.Sqrt, scale=1.0 / D, bias=EPS)
        p.op("dve", nc.vector.reciprocal, [rstd], [rstd], out=rstd[:, 0:J], in_=rstd[:, 0:J])
        for j in range(J):
            cnt["tmp"] += 1
            tm = tmps[cnt["tmp"] % 2]
            p.op("dve", nc.vector.scalar_tensor_tensor, [xt, rstd, gs], [tm], out=tm[:], in0=xt[:, j, :],
                 scalar=rstd[:, j:j + 1], in1=gs[:], op0=ALU.mult, op1=ALU.mult)
            p.tt("pool", xm, xm[:, j, :], tm, tm[:], sh, sh[:], ALU.add)
        for kc in range(8):
            tb = tbank()
            for j in range(J):
                p.mm(tb, [xm, cstb], last=(j == J - 1), transpose=True, out=tb[:, j * 128:(j + 1) * 128],
                     in_=xm[:, j, kc * 128:(kc + 1) * 128], identity=ident_b)
            if kc % 2 == 0:
                p.act(xmT, xmT[:, kc, 0:J * 128], tb, tb[:, 0:J * 128], AF.Copy)
            else:
                p.op("dve", nc.vector.tensor_copy, [tb], [xmT], out=xmT[:, kc, 0:J * 128], in_=tb[:, 0:J * 128])

    def ffn(xt, J, gate, wi_d, wo_d):
        NT = J * 128
        wir, wor, xmT, gbf, sils, t2s = FB["wir"], FB["wor"], FB["xmT"], FB["gbf"], FB["sils"], FB["t2s"]
        for i in range(NI):
            cnt["wi"] += 1
            wt = wir[cnt["wi"] % 4]
            p.dma("sp", wt, wt[:], wi_d, wi_d[i, :, :].rearrange("p (k c) -> p k c", c=256))
            ba = bank()
            for k in range(8):
                p.mm(ba, [wt, xmT], last=(k == 7), out=ba[:, 0:NT], lhsT=wt[:, k, 0:128], rhs=xmT[:, k, 0:NT], start=(k == 0), stop=(k == 7))
            bu = bank()
            for k in range(8):
                p.mm(bu, [wt, xmT], last=(k == 7), out=bu[:, 0:NT], lhsT=wt[:, k, 128:256], rhs=xmT[:, k, 0:NT], start=(k == 0), stop=(k == 7))
            cnt["sil"] += 1
            sl = sils[cnt["sil"] % 2]
            p.act(sl, sl[:, 0:NT], ba, ba[:, 0:NT], AF.Silu)
            p.op("dve", nc.vector.tensor_tensor, [sl, bu], [gbf], out=gbf[:, i, 0:NT], in0=sl[:, 0:NT], in1=bu[:, 0:NT], op=ALU.mult)
            if i % 2 == 0:
                pump(1)
        for nh in range(2):
            bos = [bank() for _ in range(J)]
            for ih in range(2):
                cnt["wo"] += 1
                wo = wor[cnt["wo"] % len(wor)]
                p.dma("sp", wo, wo[:], wo_d, wo_d[nh, :, ih * 11 * 512:(ih + 1) * 11 * 512].rearrange("p (i c) -> p i c", c=512))
                for j in range(J):
                    bo = bos[j]
                    for i2 in range(11):
                        i = ih * 11 + i2
                        p.mm(bo, [gbf, wo], last=(i2 == 10), out=bo[:, :], lhsT=gbf[:, i, j * 128:(j + 1) * 128], rhs=wo[:, i2, :],
                             start=(i == 0), stop=(i == NI - 1))
            for j in range(J):
                bo = bos[j]
                cnt["t2"] += 1
                t2 = t2s[cnt["t2"] % 2]
                p.op("dve", nc.vector.scalar_tensor_tensor, [bo, gate], [t2], out=t2[:], in0=bo[:, :], scalar=0.5,
                     in1=gate[:, nh * 512:(nh + 1) * 512], op0=ALU.mult, op1=ALU.mult)
                p.tt("pool", xt, xt[:, j, nh * 512:(nh + 1) * 512], xt, xt[:, j, nh * 512:(nh + 1) * 512], t2, t2[:], ALU.add)

    with ExitStack() as ph:
        alloc_ffn(ph, "_a")
        PC["gen"] = precast_gen(ph, 1)
        bct, xts, xmT = FB["bct"], FB["xts"], FB["xmT"]
        wgb = p.sb("wgb", [128, 8, 32], BF16, ph)
        p.dma("pool", wgb, wgb[:], wg_d, wg_d[:, :].rearrange("p (k c) -> p k c", c=32))
        gsb = [p.sb("gsb%d" % i, [32, NTL], F32, ph) for i in range(2)]
        tix = 0
        for b in range(NB):
            for seg in ("ctx", "lat"):
                bm = NB if seg == "ctx" else b
                load_mod(bm, 0, 1, 2, 0, bct["gs"], bct["sh"], bct["gate"])
                load_mod(bm, 3, 4, None, 1, bct["gs2"], bct["sh2"], None)
                ntok = CT if seg == "ctx" else T
                src = ctx_d if seg == "ctx" else x_d
                base = b * ntok
                tok0 = 0
                while tok0 < ntok:
                    NT = min(NTL, ntok - tok0)
                    J = NT // 128
                    tix += 1
                    xt = xts[tix % 2]
                    p.dma("sp", xt, xt[:, 0:J, :], src, src[base + tok0:base + tok0 + NT, :].rearrange("(j p) d -> p j d", p=128))
                    norm_mod_T(xt, J, bct["gs"], bct["sh"])
                    ffn(xt, J, bct["gate"], w1i_b, w1o_b)
                    if seg == "lat":
                        p.dma("sp", x1_s, x1_s[base + tok0:base + tok0 + NT, :].rearrange("(j p) d -> p j d", p=128), xt, xt[:, 0:J, :])
                    norm_mod_T(xt, J, bct["gs2"], bct["sh2"])
                    col0 = tok0 if seg == "ctx" else CT + tok0
                    p.dma("sp", xl_s, xl_s[b, :, :].rearrange("p (k t) -> p k t", t=TA)[:, :, col0:col0 + NT], xmT, xmT[:, :, 0:NT])
                    bk = bank()
                    for k in range(8):
                        p.mm(bk, [wgb, xmT], last=(k == 7), out=bk[0:32, 0:NT], lhsT=wgb[:, k, :], rhs=xmT[:, k, 0:NT], start=(k == 0), stop=(k == 7))
                    gs_ = gsb[tix % 2]
                    p.act(gs_, gs_[:, 0:NT], bk, bk[0:32, 0:NT], AF.Identity, extra_reads=[vecs], bias=vecs[0:32, V_BG:V_BG + 1], scale=1.0)
                    p.dma("sp", gates_s, gates_s[b, :, col0:col0 + NT], gs_, gs_[:, 0:NT])
                    tok0 += NT
        pump(1000)
        p.barrier()

    NCH = NBLK
    NBLK_L = NBLK - NBLK_C
    ttiles = []
    t0_ = 0
    while t0_ < CT:
        n_ = min(NTL, CT - t0_)
        ttiles.append((t0_, n_))
        t0_ += n_
    while t0_ < TA:
        ttiles.append((t0_, NTL))
        t0_ += NTL
    ord_f = list(range(NBLK))
    ord_b = list(range(NBLK_C - 1, -1, -1)) + list(range(NBLK - 1, NBLK_C - 1, -1))
    with ExitStack() as ph:
        GT = p.sb("GT", [128, NBLK, 128], F32, ph)
        decb = p.sb("decb", [128, 64, NCH], F32, ph)
        mgb = p.sb("mgb", [128, D], F32, ph)
        LB = p.sb("LB", [128, 16], F32, ph)
        OML = p.sb("OML", [128, 16], F32, ph)
        NOML = p.sb("NOML", [128, 16], F32, ph)
        xh = p.sb("xh", [128, 8, TA], BF16, ph)
        p.dma("sp", mgb, mgb[:], rows_d, rows_d[4:5, :].partition_broadcast(128))
        rmask4 = p.sb("rmask4", [128, 4, 128], F32, ph)
        for cc in range(4):
            p.op("dve", nc.vector.tensor_copy, [cst], [rmask4], out=rmask4[:, cc, :], in_=cst[:, 4, cc * 32 + 31:cc * 32 + 32].broadcast_to([128, 128]))
        lbv = vecs[:, V_LB:V_LB + 32].rearrange("p (d l h) -> p d l h", d=2, l=2)
        p.op("dve", nc.vector.tensor_tensor, [vecs], [LB], out=LB[:].rearrange("p (d h) -> p d h", d=2), in0=lbv[:, :, 0, :], in1=lbv[:, :, 1, :], op=ALU.subtract)
        p.act(LB, LB[:], LB, LB[:], AF.Sigmoid)
        p.op("dve", nc.vector.tensor_scalar, [LB], [OML], out=OML[:], in0=LB[:], scalar1=-1.0, scalar2=1.0, op0=ALU.mult, op1=ALU.add)
        p.op("dve", nc.vector.tensor_scalar, [OML], [NOML], out=NOML[:], in0=OML[:], scalar1=-1.0, scalar2=None, op0=ALU.mult)
        with ExitStack() as g2:
            IG = p.sb("IG", [128, TA], F32, g2)
            FG = p.sb("FG", [128, TA], F32, g2)
            Pc = p.sb("Pc", [128, TA], F32, g2)
            NBq = p.sb("NBq", [128, TA], F32, g2)
            Aa = p.sb("Aa", [128, TA], F32, g2)
            WTH = p.sb("WTH", [128, TA], BF16, g2)
            cmk = p.sb("cmk", [128, TA], F32, g2)
            tot = p.sb("tot", [128, NCH], F32, g2)
            amax = p.sb("amax", [128, NCH], F32, g2)
            mc = p.sb("mc", [128, NCH], F32, g2)
            MP = p.sb("MP", [128, NCH], F32, g2)
            dec = p.sb("dec", [64, NCH], F32, g2)
            dbig = p.sb("dbig", [64, 64, NCH], F32, g2)
            ones64 = p.sb("ones64", [64, 128], F32, g2)
            p.op("dve", nc.vector.memset, [], [IG], ap=IG[:], constant=0.0)
            p.op("dve", nc.vector.memset, [], [FG], ap=FG[:], constant=0.0)
            p.op("dve", nc.vector.memset, [], [ones64], ap=ones64[:], constant=1.0)
            p.dma("sp", cmk, cmk[0:64, :], cmask_d, cmask_d[:, :])
            p.dma("sp", cmk, cmk[64:128, :], cmask_d, cmask_d[:, :])
            for q_ in range(2):
                for dr in range(2):
                    for b in range(NB):
                        r = q_ * 64 + dr * 32 + b * 8
                        p.dma("sp", IG, IG[r:r + 8, :], gates_s, gates_s[b, dr * 8:dr * 8 + 8, :])
                        p.dma("sp", FG, FG[r:r + 8, :], gates_s, gates_s[b, 16 + dr * 8:16 + dr * 8 + 8, :])
            p.act(FG, FG[:], FG, FG[:], AF.Exp, scale=-1.0)
            p.act(FG, FG[:], FG, FG[:], AF.Ln, bias=1.0, scale=1.0)
            p.op("dve", nc.vector.tensor_tensor_scan, [cmk, FG], [Pc], out=Pc[:], data0=cmk[:], data1=FG[:], initial=0.0, op0=ALU.mult, op1=ALU.add)
            Pv = Pc[:].rearrange("p (c l) -> p c l", l=LM)
            p.op("dve", nc.vector.tensor_copy, [Pc], [tot], out=tot[:].rearrange("p (c o) -> p c o", o=1), in_=Pv[:, :, LM - 1:LM])
            for q_ in range(2):
                f0, f1, b0_, b1_ = q_ * 64, q_ * 64 + 32, q_ * 64 + 32, q_ * 64 + 64
                p.op("dve", nc.vector.tensor_copy, [Pc], [NBq], out=NBq[f0:f1, :], in_=Pc[f0:f1, :])
                p.op("dve", nc.vector.tensor_tensor, [FG, Pc], [NBq], out=NBq[b0_:b1_, :], in0=FG[b0_:b1_, :], in1=Pc[b0_:b1_, :], op=ALU.subtract)
                p.op("dve", nc.vector.tensor_tensor, [NBq, tot], [NBq], out=NBq[b0_:b1_, :].rearrange("p (c l) -> p c l", l=LM),
                     in0=NBq[b0_:b1_, :].rearrange("p (c l) -> p c l", l=LM),
                     in1=tot[b0_:b1_, :].rearrange("p (c o) -> p c o", o=1).broadcast_to([32, NCH, LM]), op=ALU.add)
            p.op("dve", nc.vector.tensor_tensor, [IG, NBq], [Aa], out=Aa[:], in0=IG[:], in1=NBq[:], op=ALU.add)
            p.op("dve", nc.vector.tensor_reduce, [Aa], [amax], out=amax[:], in_=Aa[:].rearrange("p (c l) -> p c l", l=LM), axis=AX.X, op=ALU.max)
            p.op("dve", nc.vector.memset, [], [MP], ap=MP[:], constant=0.0)
            for q_ in range(2):
                for dr, order in ((0, ord_f), (1, ord_b)):
                    rs_ = slice(q_ * 64 + dr * 32, q_ * 64 + dr * 32 + 32)
                    for j, c in enumerate(order):
                        p.op("dve", nc.vector.tensor_tensor, [amax, MP], [mc], out=mc[rs_, c:c + 1], in0=amax[rs_, c:c + 1], in1=MP[rs_, c:c + 1], op=ALU.max)
                        if j + 1 < len(order):
                            c2 = order[j + 1]
                            p.op("dve", nc.vector.tensor_tensor, [mc, tot], [MP], out=MP[rs_, c2:c2 + 1], in0=mc[rs_, c:c + 1], in1=tot[rs_, c:c + 1], op=ALU.subtract)
            p.op("dve", nc.vector.tensor_tensor, [MP, mc], [dec], out=dec[:], in0=MP[0:64, :], in1=mc[0:64, :], op=ALU.subtract)
            p.act(dec, dec[:], dec, dec[:], AF.Exp)
            mcb = mc[:].rearrange("p (c o) -> p c o", o=1).broadcast_to([128, NCH, LM])
            p.op("dve", nc.vector.tensor_tensor, [Aa, mc], [Aa], out=Aa[:].rearrange("p (c l) -> p c l", l=LM), in0=Aa[:].rearrange("p (c l) -> p c l", l=LM), in1=mcb, op=ALU.subtract)
            p.op("dve", nc.vector.tensor_tensor, [NBq, mc], [NBq], out=NBq[:].rearrange("p (c l) -> p c l", l=LM), in0=NBq[:].rearrange("p (c l) -> p c l", l=LM), in1=mcb, op=ALU.subtract)
            p.act(WTH, WTH[0:64, :], Aa, Aa[0:64, :], AF.Exp)
            p.act(WTH, WTH[64:128, :], NBq, NBq[64:128, :], AF.Exp)
            for blk in range(NBLK):
                tb = tbank()
                p.mm(tb, [WTH, cstb], transpose=True, out=tb[:, 0:128], in_=WTH[:, blk * 128:(blk + 1) * 128], identity=ident_b)
                p.op("dve", nc.vector.tensor_copy, [tb], [GT], out=GT[:, blk, :], in_=tb[:, 0:128])
            p.op("dve", nc.vector.tensor_tensor, [cst, dec], [dbig], out=dbig[:],
                 in0=cst[0:64, 0, 0:64].rearrange("p (r o) -> p r o", o=1).broadcast_to([64, 64, NCH]),
                 in1=dec[:].rearrange("p (o c) -> p o c", o=1).broadcast_to([64, 64, NCH]), op=ALU.mult)
            rper = 512 // NCH
            r0_ = 0
            while r0_ < 64:
                nr = min(rper, 64 - r0_)
                bk = bank()
                p.mm(bk, [ones64, dbig], out=bk[:, 0:nr * NCH], lhsT=ones64[:, :], rhs=dbig[:, r0_:r0_ + nr, :], start=True, stop=True)
                p.op("dve", nc.vector.tensor_copy, [bk], [decb], out=decb[:, r0_:r0_ + nr, :], in_=bk[:, 0:nr * NCH].rearrange("p (r c) -> p r c", c=NCH))
                r0_ += nr
            p.barrier()
        for b in range(NB):
          with ExitStack() as xs:
            xl = p.sb("xl", [128, 8, TA], BF16, xs)
            p.dma("sp", xl, xl[:], xl_s, xl_s[b, :, :].rearrange("p (k t) -> p k t", t=TA))
            with ExitStack() as m2:
                qT = p.sb("qT", [64, TA], BF16, m2)
                kT = p.sb("kT", [64, TA], BF16, m2)
                kv = p.sb("kv", [128, NBLK, 192], BF16, m2)
                sigo = p.sb("sigo", [128, NBLK_L, 128], BF16, m2)
                Vp = [p.sb("Vp%d" % i, [128, NBLK, 130], BF16, m2) for i in range(2)]
                Hr = [p.sb("Hr%d" % i, [128, NBLK_L, 130], F32, m2) for i in range(2)]
                Hm = p.sb("Hm", [128, NBLK_L, 128], F32, m2)
                dnn = p.sb("dnn", [128, 2, NBLK_L], F32, m2)
                hmb = p.sb("hmb", [128, NBLK_L, 128], BF16, m2)
                hmTh = p.sb("hmTh", [128, T], BF16, m2)
                ssH = p.sb("ssH", [128, NBLK_L], F32, m2)
                ATm = [p.sb("ATm%d" % i, [128, 128], BF16, m2) for i in range(4)]
                Cf = [p.sb("Cf%d" % i, [64, 130], F32, m2) for i in range(2)]
                Cd = [p.sb("Cd%d" % i, [64, 130], BF16, m2) for i in range(4)]
                wqk = [p.sb("wqk%d" % i, [128, 8, 256], BF16, m2) for i in range(2)]
                wtm = [p.sb("wtm%d" % i, [128, 8, 320], BF16, m2) for i in range(2)]
                btm = [p.sb("btm%d" % i, [1, 320], BF16, m2) for i in range(2)]
                ci = 0
                for h in range(8 if STAGE >= 2 else 0):
                    hp, hh = h // 2, h % 2
                    wq = wqk[hp % 2]
                    if hh == 0:
                        p.dma("pool", wq, wq[:], wmfm_d, wmfm_d[hp, :, :].rearrange("p (k c) -> p k c", c=256))
                    wt = wtm[h % 2]
                    bt = btm[h % 2]
                    p.dma("pool", wt, wt[:], wmtm_d, wmtm_d[h, :, :].rearrange("p (k c) -> p k c", c=320))
                    p.dma("pool", bt, bt[:], bmtm_d, bmtm_d[h, :, :])
                    for (t0, n) in ttiles:
                        bq = bank()
                        for k in range(8):
                            p.mm(bq, [wq, xl], last=(k == 7), out=bq[0:64, 0:n], lhsT=wq[:, k, hh * 64:hh * 64 + 64], rhs=xl[:, k, t0:t0 + n], start=(k == 0), stop=(k == 7))
                        bk_ = bank()
                        for k in range(8):
                            p.mm(bk_, [wq, xl], last=(k == 7), out=bk_[0:64, 0:n], lhsT=wq[:, k, 128 + hh * 64:128 + hh * 64 + 64], rhs=xl[:, k, t0:t0 + n], start=(k == 0), stop=(k == 7))
                        vq = V_BMFM + hp * 2
                        p.op("dve", nc.vector.tensor_scalar, [bq, vecs], [qT], out=qT[:, t0:t0 + n], in0=bq[0:64, 0:n],
                             scalar1=vecs[hh * 64:hh * 64 + 64, vq:vq + 1], scalar2=0.125, op0=ALU.add, op1=ALU.mult)
                        p.act(kT, kT[:, t0:t0 + n], bk_, bk_[0:64, 0:n], AF.Identity, extra_reads=[vecs], bias=vecs[hh * 64:hh * 64 + 64, vq + 1:vq + 2], scale=1.0)
                    for blk in range(NBLK):
                        bo = bank()
                        for k in range(8):
                            p.mm(bo, [wt, xl], last=False, out=bo[:, 0:320], lhsT=xl[:, k, blk * 128:(blk + 1) * 128], rhs=wt[:, k, :], start=(k == 0), stop=False)
                        p.mm(bo, [ones1, bt], out=bo[:, 0:320], lhsT=ones1[0:1, :], rhs=bt[0:1, :], start=False, stop=True)
                        p.act(kv, kv[:, blk, :], bo, bo[:, 0:192], AF.Copy)
                        if blk >= NBLK_C:
                            p.act(sigo, sigo[:, blk - NBLK_C, :], bo, bo[:, 192:320], AF.Sigmoid)
                    for dr in range(2):
                        r = dr * 32 + b * 8 + h
                        wcol = GT[:, :, r:r + 1]
                        p.op("dve", nc.vector.tensor_tensor, [kv, GT], [Vp[dr]], out=Vp[dr][:, :, 0:128], in0=kv[:, :, 64:192], in1=wcol.broadcast_to([128, NBLK, 128]), op=ALU.mult)
                        p.op("dve", nc.vector.tensor_copy, [GT], [Vp[dr]], out=Vp[dr][:, :, 128:129], in_=wcol)
                        p.op("dve", nc.vector.memset, [], [Cf[dr]], ap=Cf[dr][:], constant=0.0)
                    for j in range(NBLK):
                        fr = []
                        for dr, order in ((0, ord_f), (1, ord_b)):
                            c = order[j]
                            r = dr * 32 + b * 8 + h
                            dcol = decb[0:64, r, c:c + 1]
                            tk = slice(c * 128, (c + 1) * 128)
                            ci += 1
                            bo = None
                            if c >= NBLK_C:
                                cd = Cd[ci % 4]
                                at = ATm[ci % 4]
                                p.op("dve", nc.vector.tensor_scalar, [Cf[dr], decb], [cd], out=cd[:, 0:129], in0=Cf[dr][:, 0:129], scalar1=dcol, scalar2=None, op0=ALU.mult)
                                bs_ = bank()
                                p.mm(bs_, [kT, qT], out=bs_[:, 0:128], lhsT=kT[:, tk], rhs=qT[:, tk], start=True, stop=True)
                                p.op("dve", nc.vector.tensor_tensor, [bs_, cst], [at], out=at[:], in0=bs_[:, 0:128], in1=cst[:, 2 + dr, :], op=ALU.mult)
                                bo = bank()
                                p.mm(bo, [at, Vp[dr]], last=False, out=bo[:, 0:129], lhsT=at[:], rhs=Vp[dr][:, c, 0:129], start=True, stop=False)
                                p.mm(bo, [qT, cd], out=bo[:, 0:129], lhsT=qT[:, tk], rhs=cd[:, 0:129], start=False, stop=True)
                            bc_ = bank()
                            p.mm(bc_, [kv, Vp[dr]], out=bc_[0:64, 0:129], lhsT=kv[:, c, 0:64], rhs=Vp[dr][:, c, 0:129], start=True, stop=True)
                            fr.append((dr, c, dcol, bo, bc_))
                        for (dr, c, dcol, bo, bc_) in fr:
                            p.op("dve", nc.vector.scalar_tensor_tensor, [Cf[dr], decb, bc_], [Cf[dr]], out=Cf[dr][:, 0:129], in0=Cf[dr][:, 0:129], scalar=dcol,
                                 in1=bc_[0:64, 0:129], op0=ALU.mult, op1=ALU.add)
                            if c >= NBLK_C:
                                p.act(Hr[dr], Hr[dr][:, c - NBLK_C, 0:129], bo, bo[:, 0:129], AF.Copy)
                    for dr in range(2):
                        r = dr * 32 + b * 8 + h
                        dv_ = dnn[:, dr, :].rearrange("p (c o) -> p c o", o=1)
                        p.act(dnn, dv_, Hr[dr], Hr[dr][:, :, 128:129], AF.Abs)
                        p.op("dve", nc.vector.tensor_tensor, [dnn, GT], [dnn], out=dv_, in0=dv_, in1=GT[:, NBLK_C:NBLK, 64 + r:64 + r + 1], op=ALU.max)
                        p.op("dve", nc.vector.reciprocal, [dnn], [dnn], out=dnn[:, dr, :], in_=dnn[:, dr, :])
                        rb_ = dv_.broadcast_to([128, NBLK_L, 128])
                        if dr == 0:
                            p.op("dve", nc.vector.tensor_tensor, [Hr[0], dnn], [Hm], out=Hm[:], in0=Hr[0][:, :, 0:128], in1=rb_, op=ALU.mult)
                        else:
                            p.tt("pool", Hr[1], Hr[1][:, :, 0:128], Hr[1], Hr[1][:, :, 0:128], dnn, rb_, ALU.mult)
                            p.tt("dve", Hm, Hm[:], Hm, Hm[:], Hr[1], Hr[1][:, :, 0:128], ALU.add)
                    tH = Buf(Hr[0].t[:, :, 0:128], "tHv")
                    p.inherit(tH, [Hr[0]])
                    p.tt("dve", tH, tH[:], Hm, Hm[:], Hm, Hm[:], ALU.mult)
                    p.op("dve", nc.vector.tensor_reduce, [tH], [ssH], out=ssH[:], in_=tH[:], axis=AX.X, op=ALU.add)
                    p.act(ssH, ssH[:], ssH, ssH[:], AF.Sqrt, scale=1.0 / 128, bias=EPS)
                    p.op("dve", nc.vector.reciprocal, [ssH], [ssH], out=ssH[:], in_=ssH[:])
                    p.op("dve", nc.vector.tensor_tensor, [Hm, ssH], [tH], out=tH[:], in0=Hm[:], in1=ssH[:].rearrange("p (c o) -> p c o", o=1).broadcast_to([128, NBLK_L, 128]), op=ALU.mult)
                    p.tt("pool", tH, tH[:], tH, tH[:], mgb, mgb[:, h * 128:(h + 1) * 128].rearrange("p (o c) -> p o c", o=1).broadcast_to([128, NBLK_L, 128]), ALU.mult)
                    p.op("dve", nc.vector.tensor_tensor, [tH, sigo], [hmb], out=hmb[:], in0=tH[:], in1=sigo[:], op=ALU.mult)
                    p.inherit(Hr[0], [tH])
                    for c0 in range(0, NBLK_L, 8):
                        n8 = min(8, NBLK_L - c0)
                        tb = tbank()
                        for cl in range(c0, c0 + n8):
                            p.mm(tb, [hmb, cstb], last=(cl == c0 + n8 - 1), transpose=True, out=tb[:, (cl - c0) * 128:(cl - c0 + 1) * 128], in_=hmb[:, cl, :], identity=ident_b)
                        p.act(hmTh, hmTh[:, c0 * 128:(c0 + n8) * 128], tb, tb[:, 0:n8 * 128], AF.Copy)
                    p.dma("sp", hm_s, hm_s[b, :, h * T:(h + 1) * T], hmTh, hmTh[:])
                p.barrier()
            for k in range(8):
                e_ = "pool" if k % 2 else "dve"
                p.op(e_, p.eng_of(e_).tensor_copy, [xl], [xh], out=xh[:, k, 0:CT], in_=xl[:, k, 0:CT])
                p.op(e_, p.eng_of(e_).tensor_copy, [xl], [xh], out=xh[:, k, CT:TA].rearrange("p (c r) -> p c r", r=ROWS),
                     in_=xl[:, k, CT:TA].rearrange("p (r c) -> p c r", c=GW))
            p.barrier()
          if True:
            with ExitStack() as h2:
                vtm = p.sb("vtm", [128, NBLK, 512], BF16, h2)
                qs = p.sb("qs", [128, TA], BF16, h2)
                gsl = p.sb("gsl", [128, T], BF16, h2)
                SG = [p.sb("SG%d" % i, [128, TA], F32, h2) for i in range(2)]
                Gt = p.sb("Gt", [128, TA], F32, h2)
                kks = [p.sb("kk%d" % i, [128, TA], BF16, h2) for i in range(2)]
                qds = [p.sb("qd%d" % i, [128, TA], BF16, h2) for i in range(2)]
                kds = [p.sb("kd%d" % i, [128, TA], BF16, h2) for i in range(2)]
                hmk = p.sb("hmk", [128, TA], BF16, h2)
                totc = p.sb("totc", [128, NCHH], F32, h2)
                dSs = [p.sb("dS%d" % i, [128, NCHH], F32, h2) for i in range(2)]
                Hd = [p.sb("Hd%d" % i, [128, T], F32, h2) for i in range(2)]
                HhT = Hd[0]
                sqb = qds[0]
                hhTh = kds[0]
                rsb = [p.sb("rsb%d" % i, [128, 512], F32, h2) for i in range(2)]
                khm = [[p.sb("khm%d_%d" % (d_, i), [128, 4, 128], BF16, h2) for i in range(2)] for d_ in range(2)]
                ATh = [[p.sb("ATh%d_%d" % (d_, i), [128, 128], BF16, h2) for i in range(2)] for d_ in range(2)]
                Sfs = [[p.sb("Sf%d_%d" % (d_, i), [128, 128], F32, h2) for i in range(3)] for d_ in range(2)]
                Sbs = [[p.sb("Sb%d_%d" % (d_, i), [128, 128], BF16, h2) for i in range(4)] for d_ in range(2)]
                whf = [p.sb("whf%d" % i, [128, 8, 512], BF16, h2) for i in range(1)]
                wht = p.sb("wht", [128, 8, 512], BF16, h2)
                bht = p.sb("bht", [1, 512], BF16, h2)
                p.dma("pool", hmk, hmk[:], hmask_d, hmask_d[:, :])
                for g in range(2 if STAGE >= 3 else 0):
                    p.dma("pool", wht, wht[:], whtm_d, whtm_d[g, :, :].rearrange("p (k c) -> p k c", c=512))
                    p.dma("pool", bht, bht[:], bhtm_d, bhtm_d[g, :, :])
                    for blk in range(NBLK):
                        bo = bank()
                        for k in range(8):
                            p.mm(bo, [wht, xh], last=False, out=bo[:, :], lhsT=xh[:, k, blk * 128:(blk + 1) * 128], rhs=wht[:, k, :], start=(k == 0), stop=False)
                        p.mm(bo, [ones1, bht], out=bo[:, :], lhsT=ones1[0:1, :], rhs=bht[0:1, :], start=False, stop=True)
                        if blk % 2:
                            p.act(vtm, vtm[:, blk, :], bo, bo[:, :], AF.Copy)
                        else:
                            p.op("dve", nc.vector.tensor_copy, [bo], [vtm], out=vtm[:, blk, :], in_=bo[:, :])
                    for hl in range(4):
                        h = g * 4 + hl
                        wf = whf[0]
                        p.dma("pool", wf, wf[:], whfm_d, whfm_d[h, :, :].rearrange("p (k c) -> p k c", c=512))
                        vb = V_BHFM + h * 4
                        for (t0, n) in ttiles:
                            for part in range(4):
                                if part == 3 and t0 < CT:
                                    continue
                                bk = bank()
                                for k in range(8):
                                    p.mm(bk, [wf, xh], last=(k == 7), out=bk[:, 0:n], lhsT=wf[:, k, part * 128:(part + 1) * 128], rhs=xh[:, k, t0:t0 + n], start=(k == 0), stop=(k == 7))
                                bias = vecs[:, vb + part:vb + part + 1]
                                if part == 0:
                                    p.act(qs, qs[:, t0:t0 + n], bk, bk[:, 0:n], AF.Silu, extra_reads=[vecs], bias=bias, scale=1.0)
                                elif part == 3:
                                    p.act(gsl, gsl[:, t0 - CT:t0 - CT + n], bk, bk[:, 0:n], AF.Silu, extra_reads=[vecs], bias=bias, scale=1.0)
                                else:
                                    sg_ = SG[part - 1]
                                    p.act(sg_, sg_[:, t0:t0 + n], bk, bk[:, 0:n], AF.Sigmoid, extra_reads=[vecs], bias=bias, scale=1.0)
                        for dr in range(2):
                            sg_ = SG[dr]
                            kk, qd, kd, dS = kks[dr], qds[dr], kds[dr], dSs[dr]
                            lcol = dr * 8 + h
                            p.op("pool", nc.gpsimd.tensor_scalar, [sg_, NOML, OML], [kk], out=kk[:], in0=sg_[:], scalar1=NOML[:, lcol:lcol + 1], scalar2=OML[:, lcol:lcol + 1], op0=ALU.mult, op1=ALU.add)
                            p.op("dve", nc.vector.tensor_scalar, [sg_, OML, LB], [sg_], out=sg_[:], in0=sg_[:], scalar1=OML[:, lcol:lcol + 1], scalar2=LB[:, lcol:lcol + 1], op0=ALU.mult, op1=ALU.add)
                            p.act(sg_, sg_[:], sg_, sg_[:], AF.Ln)
                            p.op("dve", nc.vector.tensor_tensor_scan, [hmk, sg_], [Gt], out=Gt[:], data0=hmk[:], data1=sg_[:], initial=0.0, op0=ALU.mult, op1=ALU.add)
                            Gv = Gt[:].rearrange("p (c l) -> p c l", l=LH)
                            tv = totc[:].rearrange("p (c o) -> p c o", o=1)
                            p.op("dve", nc.vector.tensor_copy, [Gt], [totc], out=tv, in_=Gv[:, :, LH - 1:LH])
                            if dr == 1:
                                p.op("dve", nc.vector.tensor_tensor, [sg_, Gt], [Gt], out=Gt[:], in0=sg_[:], in1=Gt[:], op=ALU.subtract)
                                p.op("dve", nc.vector.tensor_tensor, [Gt, totc], [Gt], out=Gv, in0=Gv, in1=tv.broadcast_to([128, NCHH, LH]), op=ALU.add)
                            p.act(dS, dS[:], totc, totc[:], AF.Exp)
                            p.act(sg_, sg_[:], Gt, Gt[:], AF.Exp)
                            p.tt("pool", qd, qd[:], qs, qs[:], sg_, sg_[:], ALU.mult)
                            p.act(sg_, sg_[:], Gt, Gt[:], AF.Exp, scale=-1.0)
                            p.tt("dve", kd, kd[:], kk, kk[:], sg_, sg_[:], ALU.mult)
                            p.op("dve", nc.vector.tensor_tensor, [totc, Gt], [sg_], out=sg_[:].rearrange("p (c l) -> p c l", l=LH), in0=tv.broadcast_to([128, NCHH, LH]), in1=Gv, op=ALU.subtract)
                            p.act(sg_, sg_[:], sg_, sg_[:], AF.Exp)
                            p.tt("pool", kk, kk[:], kk, kk[:], sg_, sg_[:], ALU.mult)
                            p.op("dve", nc.vector.memset, [], [Sfs[dr][0]], ap=Sfs[dr][0][:], constant=0.0)
                            p.op("dve", nc.vector.memset, [], [Sbs[dr][0]], ap=Sbs[dr][0][:], constant=0.0)
                        sis = [0, 0]

                        def hg_front(dr, bi):
                            kk, qd, kd = kks[dr], qds[dr], kds[dr]
                            blk = (ord_f if dr == 0 else ord_b)[bi]
                            tk = slice(blk * 128, (blk + 1) * 128)
                            lat = blk >= NBLK_C
                            bx = bank()
                            p.mm(bx, [kk, cstb], out=bx[:, 0:128], lhsT=kk[:, tk], rhs=ident_b, start=True, stop=True)
                            km = khm[dr][bi % 2]
                            p.op("dve", nc.vector.tensor_tensor, [bx, rmask4], [km], out=km[:], in0=bx[:, 0:128].rearrange("p (o c) -> p o c", o=1).broadcast_to([128, 4, 128]),
                                 in1=rmask4[:], op=ALU.mult)
                            bu = bank()
                            for cc in range(4):
                                p.mm(bu, [km, vtm], last=(cc == 3), out=bu[:, cc * 128:(cc + 1) * 128], lhsT=km[:, cc, :], rhs=vtm[:, blk, hl * 128:(hl + 1) * 128], start=True, stop=True)
                            if lat:
                                p.mm(bx, [kd, qd], out=bx[:, 128:256], lhsT=kd[:, tk], rhs=qd[:, tk], start=True, stop=True)
                                at = ATh[dr][bi % 2]
                                p.op("dve", nc.vector.tensor_tensor, [bx, cst], [at], out=at[:], in0=bx[:, 128:256], in1=cst[:, 4 + dr, :], op=ALU.mult)
                            return (dr, bi, blk, lat, bx, bu)

                        def hg_back(ctx_):
                            for _ in hg_back_g(ctx_):
                                pass

                        def hg_back_g(ctx_):
                            dr, bi, blk, lat, bx, bu = ctx_
                            qd, dS = qds[dr], dSs[dr]
                            Sf, Sb = Sfs[dr], Sbs[dr]
                            corder = (0, 1, 2, 3) if dr == 0 else (3, 2, 1, 0)
                            if lat:
                                at = ATh[dr][bi % 2]
                                p.mm(bx, [vtm, at], last=False, out=bx[:, 256:384], lhsT=vtm[:, blk, hl * 128:(hl + 1) * 128], rhs=at[:], start=True, stop=False)
                            for ci_, cc in enumerate(corder):
                                c = blk * 4 + cc
                                si = sis[dr]
                                if lat:
                                    p.mm(bx, [Sb[si % 4], qd], last=(ci_ == 3), out=bx[:, 256 + cc * 32:256 + (cc + 1) * 32], lhsT=Sb[si % 4][:], rhs=qd[:, c * 32:(c + 1) * 32],
                                         start=False, stop=(ci_ == 3))
                                p.op("dve", nc.vector.scalar_tensor_tensor, [Sf[si % 3], dS, bu], [Sf[(si + 1) % 3]], out=Sf[(si + 1) % 3][:], in0=Sf[si % 3][:], scalar=dS[:, c:c + 1],
                                     in1=bu[:, cc * 128:(cc + 1) * 128], op0=ALU.mult, op1=ALU.add)
                                if ci_ % 2 == 0 and HG_POOL_CAST:
                                    p.op("pool", nc.gpsimd.tensor_copy, [Sf[(si + 1) % 3]], [Sb[(si + 1) % 4]], out=Sb[(si + 1) % 4][:], in_=Sf[(si + 1) % 3][:])
                                else:
                                    p.act(Sb[(si + 1) % 4], Sb[(si + 1) % 4][:], Sf[(si + 1) % 3], Sf[(si + 1) % 3][:], AF.Copy)
                                sis[dr] = si + 1
                                yield
                            if lat:
                                lt = slice((blk - NBLK_C) * 128, (blk - NBLK_C + 1) * 128)
                                p.act(Hd[dr], Hd[dr][:, lt], bx, bx[:, 256:384], AF.Copy)
                            yield

                        if HG_SEQ == 0:
                            seq_ = [(dr, bi) for bi in range(NBLK) for dr in range(2)]
                        else:
                            seq_ = [(dr, bi) for dr in range(2) for bi in range(NBLK)]
                        if HG_SEQ == 2:
                            for bi in range(NBLK):
                                ca = hg_front(0, bi)
                                cb = hg_front(1, bi)
                                ga, gb = hg_back_g(ca), hg_back_g(cb)
                                for _ in range(5):
                                    next(ga, None)
                                    next(gb, None)
                        elif HG_PIPE:
                            pend = hg_front(*seq_[0])
                            for qi in range(len(seq_)):
                                nxt = hg_front(*seq_[qi + 1]) if qi + 1 < len(seq_) else None
                                hg_back(pend)
                                pend = nxt
                        else:
                            for qi in range(len(seq_)):
                                hg_back(hg_front(*seq_[qi]))
                        p.tt("pool", Hd[0], Hd[0][:], Hd[0], Hd[0][:], Hd[1], Hd[1][:], ALU.add)
                        p.act(sqb, sqb[:, 0:T], HhT, HhT[:], AF.Square)
                        ncol = NTL // ROWS
                        hv = hhTh[:, 0:T].rearrange("p (r c) -> p c r", c=GW)
                        for ti, t0 in enumerate(range(0, T, NTL)):
                            bk = bank()
                            p.mm(bk, [cstb, sqb], out=bk[:, :], lhsT=ones_b, rhs=sqb[:, t0:t0 + NTL], start=True, stop=True)
                            rs_ = rsb[ti % 2]
                            p.act(rs_, rs_[:], bk, bk[:, :], AF.Sqrt, scale=1.0 / 128, bias=EPS)
                            p.op("dve", nc.vector.reciprocal, [rs_], [rs_], out=rs_[:], in_=rs_[:])
                            p.op("dve", nc.vector.scalar_tensor_tensor, [HhT, vecs, rs_], [rs_], out=rs_[:], in0=HhT[:, t0:t0 + NTL], scalar=vecs[:, V_HN + h:V_HN + h + 1], in1=rs_[:], op0=ALU.mult, op1=ALU.mult)
                            c0 = t0 // ROWS
                            p.tt("dve", hhTh, hv[:, c0:c0 + ncol, :], rs_, rs_[:].rearrange("p (c r) -> p c r", r=ROWS), gsl, gsl[:, t0:t0 + NTL].rearrange("p (c r) -> p c r", r=ROWS), ALU.mult)
                        p.dma("sp", hh_s, hh_s[b, :, h * T:(h + 1) * T], hhTh, hhTh[:, 0:T])
                p.barrier()

    with ExitStack() as ph:
        alloc_ffn(ph, "_c")
        bct, xts, xmT, gbf, t2s = FB["bct"], FB["xts"], FB["xmT"], FB["gbf"], FB["t2s"]
        ss, rstd, junk = FB["ss"], FB["rstd"], FB["junk"]
        zbf = Buf(gbf.t[:, 0:8, :], "zbf")
        hmT = Buf(gbf.t[:, 8:16, :], "hmT")
        hhT = p.sb("hhT", [128, 8, NTL], BF16, ph)
        xlT = xmT
        sgs = [p.sb("sg%d" % i, [128, NTL], F32, ph) for i in range(2)]
        z1s = [p.sb("z1_%d" % i, [128, NTL], F32, ph) for i in range(2)]
        wmgr = [p.sb("wmgr%d" % i, [128, 8, 256], BF16, ph) for i in range(2)]
        wpjr = [p.sb("wpjr%d" % i, [128, 8, 256], BF16, ph) for i in range(2)]
        gatem = bct["gs2"]
        fin = bct["sh2"]
        bc_load(fin, rows_d, rows_d[3:4, :])
        tix = 0
        for b in range(NB):
            bc_load(gatem, modd, modd[b:b + 1, 5 * D:6 * D])
            load_mod(b, 6, 7, 8, 2, bct["gs"], bct["sh"], bct["gate"])
            for tok0 in range(0, T, NTL):
                NT = NTL
                J = 4
                tix += 1
                xt = xts[tix % 2]
                r0 = b * T + tok0
                p.dma("sp", xt, xt[:, 0:J, :], x1_s, x1_s[r0:r0 + NT, :].rearrange("(j p) d -> p j d", p=128))
                p.inherit(zbf, [gbf])
                p.inherit(hmT, [gbf])
                p.dma("sp", hmT, hmT[:], hm_s, hm_s[b, :, :].rearrange("p (k t) -> p k t", t=T)[:, :, tok0:tok0 + NT])
                p.dma("sp", hhT, hhT[:], hh_s, hh_s[b, :, :].rearrange("p (k t) -> p k t", t=T)[:, :, tok0:tok0 + NT])
                p.dma("sp", xlT, xlT[:], xl_s, xl_s[b, :, :].rearrange("p (k t) -> p k t", t=TA)[:, :, CT + tok0:CT + tok0 + NT])
                for mc in range(8):
                    wg_ = wmgr[mc % 2]
                    wp_ = wpjr[mc % 2]
                    p.dma("sp", wg_, wg_[:], wmg_b, wmg_b[mc, :, :].rearrange("p (k c) -> p k c", c=256))
                    p.dma("sp", wp_, wp_[:], wpj_b, wpj_b[mc, :, :].rearrange("p (k c) -> p k c", c=256))
                    for which in range(2):
                        src_h = hmT if which == 0 else hhT
                        bg = bank()
                        for k in range(8):
                            p.mm(bg, [wg_, xlT], last=(k == 7), out=bg[:, :], lhsT=wg_[:, k, which * 128:(which + 1) * 128], rhs=xlT[:, k, :],
                                 start=(k == 0), stop=(k == 7))
                        by = bank()
                        for k in range(8):
                            p.mm(by, [wp_, src_h], last=(k == 7), out=by[:, :], lhsT=wp_[:, k, which * 128:(which + 1) * 128], rhs=src_h[:, k, :],
                                 start=(k == 0), stop=(k == 7))
                        sg = sgs[which]
                        vcol = V_BMG + mc * 2 + which
                        p.act(sg, sg[:], bg, bg[:, :], AF.Sigmoid, extra_reads=[vecs], bias=vecs[:, vcol:vcol + 1], scale=1.0)
                        z1 = z1s[mc % 2]
                        if which == 0:
                            p.op("dve", nc.vector.tensor_tensor, [sg, by], [z1], out=z1[:], in0=sg[:], in1=by[:, :], op=ALU.mult)
                        else:
                            p.op("dve", nc.vector.tensor_tensor, [sg, by], [sg], out=sg[:], in0=sg[:], in1=by[:, :], op=ALU.mult)
                            p.tt("pool", zbf, zbf[:, mc, :], sg, sg[:], z1, z1[:], ALU.add)
                for nh in range(2):
                    cnt["wo"] += 1
                    wo = FB["wor"][cnt["wo"] % 3]
                    p.dma("sp", wo, wo[:, 0:8, :], wmo_b, wmo_b[nh, :, :].rearrange("p (k c) -> p k c", c=512))
                    for j in range(J):
                        bo = bank()
                        for k in range(8):
                            p.mm(bo, [zbf, wo], last=(k == 7), out=bo[:, :], lhsT=zbf[:, k, j * 128:(j + 1) * 128], rhs=wo[:, k, :],
                                 start=(k == 0), stop=(k == 7))
                        cnt["t2"] += 1
                        t2 = t2s[cnt["t2"] % 2]
                        p.op("dve", nc.vector.tensor_tensor, [bo, gatem], [t2], out=t2[:], in0=bo[:, :], in1=gatem[:, nh * 512:(nh + 1) * 512], op=ALU.mult)
                        p.tt("pool", xt, xt[:, j, nh * 512:(nh + 1) * 512], xt, xt[:, j, nh * 512:(nh + 1) * 512], t2, t2[:], ALU.add)
                p.inherit(gbf, [zbf, hmT])
                norm_mod_T(xt, J, bct["gs"], bct["sh"])
                ffn(xt, J, bct["gate"], w2i_b, w2o_b)
                p.op("dve", nc.vector.memset, [], [ss], ap=ss[:], constant=0.0)
                for j in range(J):
                    p.act(junk, junk[:], xt, xt[:, j, :], AF.Square, extra_reads=[ss], accum_out=ss[:, j:j + 1])
                    ss.w = [("act", p.cnt["act"])]
                p.act(rstd, rstd[:, 0:J], ss, ss[:, 0:J], AF.Sqrt, scale=1.0 / D, bias=EPS)
                p.op("dve", nc.vector.reciprocal, [rstd], [rstd], out=rstd[:, 0:J], in_=rstd[:, 0:J])
                for j in range(J):
                    p.op("dve", nc.vector.scalar_tensor_tensor, [xt, rstd, fin], [xt], out=xt[:, j, :], in0=xt[:, j, :],
                         scalar=rstd[:, j:j + 1], in1=fin[:], op0=ALU.mult, op1=ALU.mult)
                p.dma("sp", out_d, out_d[r0:r0 + NT, :].rearrange("(j p) d -> p j d", p=128), xt, xt[:, 0:J, :])
    p.barrier()
    return nc, p


def _tile_k(W):
    C = W.shape[1]
    return np.ascontiguousarray(W.reshape(8, 128, C).transpose(1, 0, 2)).reshape(128, 8 * C)


def _tile_w_in(W):
    a = W[:, :FH].reshape(8, 128, NI, 128)
    u = W[:, FH:].reshape(8, 128, NI, 128)
    t = np.concatenate([a, u], axis=3)
    return np.ascontiguousarray(t.transpose(2, 1, 0, 3)).reshape(NI, 128, 2048)


def _tile_w_out(W):
    t = W.reshape(NI, 128, 2, 512)
    return np.ascontiguousarray(t.transpose(2, 1, 0, 3)).reshape(2, 128, NI * 512)


def prep_shared(inp, T, CT):
    f = lambda a: np.asarray(a, dtype=np.float32)
    TA = CT + T
    W = f(inp["mix_w_in"])[0]
    bI = f(inp["mix_b_in"])[0]
    sh = {}
    sh["ada_w"] = np.ascontiguousarray(f(inp["ada_w"])[0])
    sh["ada_b"] = np.ascontiguousarray(f(inp["ada_b"])[0][None, :])
    sh["rows"] = np.stack([f(inp["ffn1_norm"])[0], f(inp["mix_norm"])[0], f(inp["ffn2_norm"])[0], f(inp["final_norm"]), f(inp["mlstm_norm"])[0]])
    vec = np.zeros((128, NVEC), np.float32)
    vec[0:16, V_BG] = bI[O_IG:O_IG + 16]
    vec[16:32, V_BG] = bI[O_FG:O_FG + 16]
    for hp in range(4):
        vec[:, V_BMFM + hp * 2 + 0] = bI[O_MQ + hp * 128:O_MQ + (hp + 1) * 128]
        vec[:, V_BMFM + hp * 2 + 1] = bI[O_MK + hp * 128:O_MK + (hp + 1) * 128]
    for h in range(8):
        vec[:, V_BHFM + h * 4 + 0] = bI[O_HQ + h * 128:O_HQ + (h + 1) * 128]
        vec[:, V_BHFM + h * 4 + 1] = bI[O_HF + h * 128:O_HF + (h + 1) * 128]
        vec[:, V_BHFM + h * 4 + 2] = bI[O_HF + 1024 + h * 128:O_HF + 1024 + (h + 1) * 128]
        vec[:, V_BHFM + h * 4 + 3] = bI[O_HG + h * 128:O_HG + (h + 1) * 128]
        vec[:, V_HN + h] = f(inp["hgrn_norm"])[0][h * 128:(h + 1) * 128]
    for mc in range(8):
        vec[:, V_BMG + mc * 2 + 0] = bI[O_GM + mc * 128:O_GM + (mc + 1) * 128]
        vec[:, V_BMG + mc * 2 + 1] = bI[O_GH + mc * 128:O_GH + (mc + 1) * 128]
    lbl = f(inp["hgrn_lb_logits"])
    for d_ in range(2):
        for l_ in range(2):
            for h in range(8):
                vec[:, V_LB + d_ * 16 + l_ * 8 + h] = lbl[d_, l_, h * 128:(h + 1) * 128]
    sh["vecs"] = vec
    cst = np.zeros((128, 6, 128), np.float32)
    cst[:, 0] = np.eye(128)
    cst[:, 1] = 1.0
    s_ = np.arange(128)[:, None]
    t_ = np.arange(128)[None, :]
    cst[:, 2] = (s_ <= t_)
    cst[:, 3] = (s_ >= t_)
    same = (s_ // 32) == (t_ // 32)
    cst[:, 4] = (s_ <= t_) & same
    cst[:, 5] = (s_ >= t_) & same
    sh["consts"] = cst.reshape(128, 6 * 128)
    cm = np.ones((64, TA), np.float32)
    cm[:, ::128] = 0.0
    sh["cmask"] = cm
    hm = np.ones((128, TA), np.float32)
    hm[:, ::32] = 0.0
    sh["hmask"] = hm
    sel = np.zeros((8, 8, 64), np.float32)
    for h in range(8):
        sel[h, h, :] = 1.0
    sh["sel"] = sel.reshape(8, 8 * 64)
    sh["w1i"] = _tile_w_in(f(inp["ffn1_w_in"])[0])
    sh["w1o"] = _tile_w_out(f(inp["ffn1_w_out"])[0])
    sh["w2i"] = _tile_w_in(f(inp["ffn2_w_in"])[0])
    sh["w2o"] = _tile_w_out(f(inp["ffn2_w_out"])[0])
    sh["wg"] = _tile_k(np.concatenate([W[:, O_IG:O_IG + 16], W[:, O_FG:O_FG + 16]], 1))
    sh["wmfm"] = np.stack([_tile_k(np.concatenate([W[:, O_MQ + hp * 128:O_MQ + (hp + 1) * 128], W[:, O_MK + hp * 128:O_MK + (hp + 1) * 128]], 1)) for hp in range(4)])
    sh["wmtm"] = np.stack([_tile_k(np.concatenate([W[:, O_MK + h * 64:O_MK + (h + 1) * 64], W[:, O_MV + h * 128:O_MV + (h + 1) * 128],
                                                   W[:, O_MO + h * 128:O_MO + (h + 1) * 128]], 1)) for h in range(8)])
    sh["bmtm"] = np.stack([np.concatenate([bI[O_MK + h * 64:O_MK + (h + 1) * 64], bI[O_MV + h * 128:O_MV + (h + 1) * 128],
                                           bI[O_MO + h * 128:O_MO + (h + 1) * 128]])[None, :] for h in range(8)])
    sh["whfm"] = np.stack([_tile_k(np.concatenate([W[:, O_HQ + h * 128:O_HQ + (h + 1) * 128], W[:, O_HF + h * 128:O_HF + (h + 1) * 128],
                                                   W[:, O_HF + 1024 + h * 128:O_HF + 1024 + (h + 1) * 128],
                                                   W[:, O_HG + h * 128:O_HG + (h + 1) * 128]], 1)) for h in range(8)])
    sh["whtm"] = np.stack([_tile_k(W[:, O_HI + g * 512:O_HI + (g + 1) * 512]) for g in range(2)])
    sh["bhtm"] = np.stack([bI[O_HI + g * 512:O_HI + (g + 1) * 512][None, :] for g in range(2)])
    pm = f(inp["proj_m"])[0]
    ph_ = f(inp["proj_h"])[0]
    sh["wmg"] = np.stack([_tile_k(np.concatenate([W[:, O_GM + mc * 128:O_GM + (mc + 1) * 128], W[:, O_GH + mc * 128:O_GH + (mc + 1) * 128]], 1)) for mc in range(8)])
    sh["wpj"] = np.stack([_tile_k(np.concatenate([pm[:, mc * 128:(mc + 1) * 128], ph_[:, mc * 128:(mc + 1) * 128]], 1)) for mc in range(8)])
    wo = f(inp["mix_w_out"])[0]
    sh["wmo"] = np.stack([_tile_k(wo[:, nh * 512:(nh + 1) * 512]) for nh in range(2)])
    return {k: np.ascontiguousarray(v, dtype=np.float32) for k, v in sh.items()}


def prep_core(inp, bs):
    f = lambda a: np.asarray(a, dtype=np.float32)
    NB = len(bs)
    x = f(inp["x"])[bs]
    cx = f(inp["ctx"])[bs]
    cmat = np.concatenate([f(inp["c"])[bs], f(inp["c_ctx"])[None, :]], 0)
    cc = np.ascontiguousarray(cmat.reshape(NB + 1, 8, 128).transpose(2, 1, 0)).reshape(128, 8 * (NB + 1))
    return {"x": np.ascontiguousarray(x.reshape(-1, D)), "ctx": np.ascontiguousarray(cx.reshape(-1, D)), "cc": cc}


_CACHE = {}


def kernel(**inputs):
    B, T, _ = inputs["x"].shape
    CT = inputs["ctx"].shape[1]
    NB = B // NCORES
    key = (NB, T, CT)
    if key not in _CACHE:
        _CACHE[key] = build(NB, T, CT)[0]
    nc = _CACHE[key]
    shared = prep_shared(inputs, T, CT)
    in_maps = []
    for c in range(NCORES):
        m = dict(shared)
        m.update(prep_core(inputs, list(range(c * NB, (c + 1) * NB))))
        in_maps.append(m)
    res = run_bass_kernel_spmd(nc, in_maps, core_ids=list(range(NCORES)))
    out = np.concatenate([np.asarray(r["out"]).reshape(NB, T, D) for r in res.results], axis=0)
    return out.astype(np.float32)
```
